# Optimizing a Trainium2 kernel written in Bass

```python
import math
import jax
import jax.numpy as jnp
from jax import lax
import numpy as np

D_MODEL = 1024
BATCH = 4
SEQ = 4096
DEPTH = 1

MIX_W = D_MODEL
RET_HEADS = 4
RET_V_W = MIX_W // 2
RET_VAL_DIM = RET_V_W // RET_HEADS
RET_KEY_DIM = RET_VAL_DIM // 2
RET_QK_W = RET_HEADS * RET_KEY_DIM
RET_CHUNK = 128
DIFF_HEADS = 4
DIFF_V_W = MIX_W - RET_V_W
DIFF_HEAD_DIM = DIFF_V_W // (2 * DIFF_HEADS)
DIFF_QK_W = DIFF_HEADS * 2 * DIFF_HEAD_DIM
Q_BLOCK = 128
IN_SIZES = (RET_QK_W, RET_QK_W, RET_V_W, RET_V_W, DIFF_QK_W, DIFF_QK_W, DIFF_V_W)
IN_W = sum(IN_SIZES)
REL_BUCKETS = 32
REL_MAX_DIST = 128
N_EXPERTS = 32
TOP_K = 4
D_FF = D_MODEL
SWIGLU_ALPHA = 1.702
SWIGLU_LIMIT = 7.0
MOE_BLOCK = 128
NORM_EPS = 1e-6

kernel_name = "hymba_style_retention_diffattn_moe_adaln"


def rms_norm(x, w=None):
    xf = x.astype(jnp.float32)
    y = xf * lax.rsqrt(jnp.mean(xf * xf, axis=-1, keepdims=True) + NORM_EPS)
    if w is not None:
        y = y * w
    return y.astype(x.dtype)


def rotate_every_two(x):
    x1 = x[..., ::2]
    x2 = x[..., 1::2]
    return jnp.stack((-x2, x1), axis=-1).reshape(x.shape)


def retention(q, k, v):
    B, S, H, dk = q.shape
    dv = v.shape[-1]
    C = RET_CHUNK
    N = S // C
    pos = jnp.arange(S, dtype=jnp.float32)
    inv_freq = 1.0 / (10000.0 ** jnp.linspace(0.0, 1.0, dk // 2, dtype=jnp.float32))
    ang = pos[:, None] * jnp.repeat(inv_freq, 2)[None, :]
    sin = jnp.sin(ang)[:, None, :].astype(q.dtype)
    cos = jnp.cos(ang)[:, None, :].astype(q.dtype)
    q = q * cos + rotate_every_two(q) * sin
    k = (k * cos + rotate_every_two(k) * sin) * (dk ** -0.5)
    log_g = jnp.log(1.0 - 2.0 ** (-5.0 - jnp.arange(H, dtype=jnp.float32)))
    i = jnp.arange(C, dtype=jnp.float32)
    rel = i[:, None] - i[None, :]
    dmask = jnp.where(rel[None] >= 0,
                      jnp.exp(jnp.maximum(rel, 0.0)[None] * log_g[:, None, None]),
                      0.0).astype(q.dtype)
    zeta = jnp.exp((C - 1.0 - i)[:, None] * log_g[None, :]).astype(q.dtype)
    xi = jnp.exp((i + 1.0)[:, None] * log_g[None, :]).astype(q.dtype)
    g_chunk = jnp.exp(C * log_g).astype(q.dtype)[:, None, None]
    qc = q.reshape(B, N, C, H, dk)
    kc = k.reshape(B, N, C, H, dk)
    vc = v.reshape(B, N, C, H, dv)
    scores = jnp.einsum('bnihd,bnjhd->bnhij', qc, kc) * dmask
    inner = jnp.einsum('bnhij,bnjhe->bnihe', scores, vc)
    kv = jnp.einsum('bnjhd,bnjhe->nbhde', kc * zeta[:, :, None], vc)

    def step(state, kv_n):
        return state * g_chunk + kv_n, state

    _, prev = lax.scan(step, jnp.zeros((B, H, dk, dv), kv.dtype), kv)
    cross = jnp.einsum('bnihd,nbhde->bnihe', qc * xi[:, :, None], prev)
    return (inner + cross).reshape(B, S, H, dv)


def t5_bucket(rel):
    n = jnp.maximum(rel, 0)
    max_exact = REL_BUCKETS // 2
    nf = jnp.maximum(n, 1).astype(jnp.float32)
    large = max_exact + (jnp.log(nf / max_exact) / math.log(REL_MAX_DIST / max_exact)
                         * (REL_BUCKETS - max_exact)).astype(jnp.int32)
    large = jnp.minimum(large, REL_BUCKETS - 1)
    return jnp.where(n < max_exact, n, large)


def diff_attention(q, k, v, lam, rel_bias):
    B, S, H, _, d = q.shape
    N = S // Q_BLOCK
    q = q * (d ** -0.5)
    qb = jnp.moveaxis(q.reshape(B, N, Q_BLOCK, H, 2, d), 1, 0)
    starts = jnp.arange(N, dtype=jnp.int32) * Q_BLOCK
    k_pos = jnp.arange(S, dtype=jnp.int32)

    def block(args):
        qblk, start = args
        q_pos = start + jnp.arange(Q_BLOCK, dtype=jnp.int32)
        rel = q_pos[:, None] - k_pos[None, :]
        bias = jnp.transpose(rel_bias[t5_bucket(rel)], (2, 0, 1)).astype(jnp.float32)
        s = jnp.einsum('bqhmd,bkhmd->bhmqk', qblk, k).astype(jnp.float32)
        s = s + bias[None, :, None]
        s = jnp.where(rel[None, None, None] >= 0, s, -jnp.inf)
        p = jax.nn.softmax(s, axis=-1)
        a = p[:, :, 0] - lam * p[:, :, 1]
        return jnp.einsum('bhqk,bkhe->bqhe', a.astype(v.dtype), v)

    out = lax.map(block, (qb, starts))
    return jnp.moveaxis(out, 0, 1).reshape(B, S, H, 2 * d)


def swiglu_clamped(h):
    glu = jnp.minimum(h[..., ::2], SWIGLU_LIMIT)
    lin = jnp.clip(h[..., 1::2], -SWIGLU_LIMIT, SWIGLU_LIMIT)
    return glu * jax.nn.sigmoid(SWIGLU_ALPHA * glu) * (lin + 1.0)


def moe_ffn(h, w_router, b_router, w1, b1, w2, b2):
    B, S, D = h.shape
    T = B * S
    xt = h.reshape(T, D)
    logits = (xt @ w_router + b_router).astype(jnp.float32)
    top_v, top_i = lax.top_k(logits, TOP_K)
    gate = jax.nn.softmax(top_v, axis=-1)
    TK = T * TOP_K
    flat_e = top_i.reshape(TK)
    flat_tok = jnp.arange(TK, dtype=jnp.int32) // TOP_K
    flat_w = gate.reshape(TK)
    order = jnp.argsort(flat_e)
    sorted_e = flat_e[order]
    counts = jnp.bincount(flat_e, length=N_EXPERTS)
    padded = (counts + MOE_BLOCK - 1) // MOE_BLOCK * MOE_BLOCK
    pad_end = jnp.cumsum(padded)
    pad_start = pad_end - padded
    sort_start = jnp.cumsum(counts) - counts
    rank = jnp.arange(TK, dtype=jnp.int32) - sort_start[sorted_e]
    dest = pad_start[sorted_e] + rank
    P = TK + N_EXPERTS * MOE_BLOCK
    nb = P // MOE_BLOCK
    slot_tok = jnp.zeros((P,), jnp.int32).at[dest].set(flat_tok[order])
    slot_w = jnp.zeros((P,), jnp.float32).at[dest].set(flat_w[order])
    block_e = jnp.minimum(
        jnp.searchsorted(pad_end, jnp.arange(nb, dtype=pad_end.dtype) * MOE_BLOCK, side='right'),
        N_EXPERTS - 1)
    xs = xt[slot_tok].reshape(nb, MOE_BLOCK, D)

    def expert_block(args):
        xb, e = args
        hid = xb @ w1[e] + b1[e]
        return swiglu_clamped(hid) @ w2[e] + b2[e]

    ys = lax.map(expert_block, (xs, block_e)).reshape(P, D)
    out = jax.ops.segment_sum(ys.astype(jnp.float32) * slot_w[:, None], slot_tok, num_segments=T)
    return out.astype(h.dtype).reshape(B, S, D)


def setup_inputs(seed: int = 0) -> dict:
    key = jax.random.key(seed)
    ks = jax.random.split(key, 22)
    D, E, F = D_MODEL, N_EXPERTS, D_FF
    nrm = lambda k, shape, s: jax.random.normal(k, shape, jnp.float32) * s
    return {
        "x": nrm(ks[0], (BATCH, SEQ, D), 1.0),
        "c": nrm(ks[1], (BATCH, D), 1.0),
        "w_ada": nrm(ks[2], (DEPTH, D, 6 * D), 0.5 * D ** -0.5),
        "b_ada": nrm(ks[3], (DEPTH, 6 * D), 0.01),
        "norm1_w": 1.0 + nrm(ks[4], (DEPTH, D), 0.02),
        "w_in": nrm(ks[5], (DEPTH, D, IN_W), D ** -0.5),
        "lam_q1": nrm(ks[6], (DEPTH, DIFF_HEAD_DIM), 0.1),
        "lam_k1": nrm(ks[7], (DEPTH, DIFF_HEAD_DIM), 0.1),
        "lam_q2": nrm(ks[8], (DEPTH, DIFF_HEAD_DIM), 0.1),
        "lam_k2": nrm(ks[9], (DEPTH, DIFF_HEAD_DIM), 0.1),
        "subln_w": 1.0 + nrm(ks[10], (DEPTH, 2 * DIFF_HEAD_DIM), 0.02),
        "rel_bias": nrm(ks[11], (REL_BUCKETS, DIFF_HEADS), 0.5),
        "w_out": nrm(ks[12], (DEPTH, MIX_W, D), MIX_W ** -0.5),
        "norm2_w": 1.0 + nrm(ks[13], (DEPTH, D), 0.02),
        "w_router": nrm(ks[14], (DEPTH, D, E), D ** -0.5),
        "b_router": nrm(ks[15], (DEPTH, E), 0.01),
        "w1": nrm(ks[16], (DEPTH, E, D, 2 * F), D ** -0.5),
        "b1": nrm(ks[17], (DEPTH, E, 2 * F), 0.01),
        "w2": nrm(ks[18], (DEPTH, E, F, D), F ** -0.5),
        "b2": nrm(ks[19], (DEPTH, E, D), 0.01),
        "normf_w": 1.0 + nrm(ks[20], (D,), 0.02),
    }


def reference(x, c, w_ada, b_ada, norm1_w, w_in, lam_q1, lam_k1, lam_q2, lam_k2, subln_w,
              rel_bias, w_out, norm2_w, w_router, b_router, w1, b1, w2, b2, normf_w):
    B, S, D = x.shape
    split_idx = list(np.cumsum(IN_SIZES)[:-1])
    cond = jax.nn.silu(c)
    for l in range(DEPTH):
        mod = (cond @ w_ada[l] + b_ada[l]).reshape(B, 6, D)
        shift1, scale1, gate1, shift2, scale2, gate2 = [mod[:, j][:, None, :] for j in range(6)]

        h = rms_norm(x, norm1_w[l]) * (1.0 + scale1) + shift1
        proj = h @ w_in[l]
        q_r, k_r, v_r, g_r, q_d, k_d, v_d = jnp.split(proj, split_idx, axis=-1)

        y_r = retention(q_r.reshape(B, S, RET_HEADS, RET_KEY_DIM),
                        k_r.reshape(B, S, RET_HEADS, RET_KEY_DIM),
                        v_r.reshape(B, S, RET_HEADS, RET_VAL_DIM))
        y_r = jax.nn.silu(g_r) * rms_norm(y_r).reshape(B, S, RET_V_W)

        lambda_init = 0.8 - 0.6 * math.exp(-0.3 * l)
        lam = (jnp.exp(jnp.sum(lam_q1[l] * lam_k1[l]).astype(jnp.float32))
               - jnp.exp(jnp.sum(lam_q2[l] * lam_k2[l]).astype(jnp.float32)) + lambda_init)
        y_d = diff_attention(q_d.reshape(B, S, DIFF_HEADS, 2, DIFF_HEAD_DIM),
                             k_d.reshape(B, S, DIFF_HEADS, 2, DIFF_HEAD_DIM),
                             v_d.reshape(B, S, DIFF_HEADS, 2 * DIFF_HEAD_DIM),
                             lam, rel_bias)
        y_d = (rms_norm(y_d, subln_w[l]) * (1.0 - lambda_init)).reshape(B, S, DIFF_V_W)

        mixed = jnp.concatenate([y_r.astype(x.dtype), y_d.astype(x.dtype)], axis=-1) @ w_out[l]
        x = x + gate1 * mixed

        h2 = rms_norm(x, norm2_w[l]) * (1.0 + scale2) + shift2
        x = x + gate2 * moe_ffn(h2, w_router[l], b_router[l], w1[l], b1[l], w2[l], b2[l])
    return rms_norm(x, normf_w)
```

```python
import math
import os
from contextlib import ExitStack

import numpy as np
import concourse.bass as bass
import concourse.mybir as mybir
from concourse.bass_utils import run_bass_kernel_spmd

F32 = mybir.dt.float32
BF16 = mybir.dt.bfloat16
AF = mybir.ActivationFunctionType
ALU = mybir.AluOpType
AX = mybir.AxisListType

D = 1024
SEQ = 4096
NBLK = 32
NOWN = 16
TOWN = 2048
NE = 32
NEG = -30000.0
EPS = 1e-6
ALPHA = 1.702
LAMBDA_INIT = 0.8 - 0.6 * math.exp(-0.3 * 0)


class Sched:
    ENGS = ("pe", "act", "dve", "pool", "sp")

    def __init__(self, nc):
        self.nc = nc
        self.prog = {e: [] for e in self.ENGS}
        self.cnt = {}
        self.seen = {e: {} for e in self.ENGS}
        self.lastw = {}
        self.readers = {}
        self.sem_names = set()

    def _deps(self, reads, writes):
        deps = []
        for k in reads:
            t = self.lastw.get(k)
            if t is not None:
                deps.append(t)
        for k in writes:
            t = self.lastw.get(k)
            if t is not None:
                deps.append(t)
            deps.extend(self.readers.get(k, ()))
        return deps

    def _commit(self, ticket, reads, writes):
        for k in reads:
            self.readers.setdefault(k, []).append(ticket)
        for k in writes:
            self.lastw[k] = ticket
            self.readers[k] = []

    def _filter(self, eng, deps):
        best = {}
        for (s, v) in deps:
            if s == "E:pe" and eng == "pe":
                continue
            if v > best.get(s, 0):
                best[s] = v
        out = []
        seen = self.seen[eng]
        for s, v in best.items():
            if seen.get(s, 0) >= v:
                continue
            seen[s] = v
            out.append((s, v))
        return out

    def op(self, eng, fn, reads=(), writes=()):
        waits = self._filter(eng, self._deps(reads, writes))
        s = "E:" + eng
        self.sem_names.add(s)
        self.cnt[s] = self.cnt.get(s, 0) + 1
        ticket = (s, self.cnt[s])
        self.prog[eng].append((waits, fn, (s, 1)))
        self._commit(ticket, reads, writes)
        return ticket

    def dma(self, q, sem, fn, reads=(), writes=()):
        waits = self._filter(q, self._deps(reads, writes))
        s = "D:" + sem
        self.sem_names.add(s)
        self.cnt[s] = self.cnt.get(s, 0) + 16
        ticket = (s, self.cnt[s])
        self.prog[q].append((waits, fn, (s, 16)))
        self._commit(ticket, reads, writes)
        return ticket

    def fence(self):
        allt = list(self.cnt.items())
        for eng in self.ENGS:
            self.prog[eng].append((self._filter(eng, allt), None, None))

    def wait_all(self, eng, keys):
        deps = [self.lastw[k] for k in keys if k in self.lastw]
        self.prog[eng].append((self._filter(eng, deps), None, None))

    def emit(self):
        nc = self.nc
        with ExitStack() as st:
            sems = {}
            for s in sorted(self.sem_names):
                sems[s] = st.enter_context(nc.semaphore(s.replace(":", "_")))
            block = st.enter_context(nc.Block())

            def run(name):
                def body(eng):
                    for waits, fn, inc in self.prog[name]:
                        for (s, v) in waits:
                            eng.wait_ge(sems[s], v)
                        if fn is not None:
                            fn(eng).then_inc(sems[inc[0]], inc[1])
                return body

            block.tensor(run("pe"))
            block.scalar(run("act"))
            block.vector(run("dve"))
            block.gpsimd(run("pool"))
            block.sync(run("sp"))


def own_blocks(half):
    r = (0, 3) if half == 0 else (1, 2)
    own = [j for j in range(NBLK) if j % 4 in r]
    oth = [j for j in range(NBLK) if j % 4 not in r]
    return own, oth


def t5_bucket_np(rel):
    n = np.maximum(rel, 0)
    nf = np.maximum(n, 1).astype(np.float32)
    large = 16 + (np.log(nf / np.float32(16)) / np.float32(math.log(128 / 16)) * np.float32(16)).astype(np.int32)
    large = np.minimum(large, 31)
    return np.where(n < 16, n, large)


def const_tables(half):
    own, oth = own_blocks(half)
    perm = own + oth
    pos = np.concatenate([np.arange(j * 128, (j + 1) * 128) for j in perm]).astype(np.float32)
    inv_freq = (1.0 / (10000.0 ** np.linspace(0.0, 1.0, 32, dtype=np.float32))).astype(np.float32)
    ang = pos[:, None] * np.repeat(inv_freq, 2)[None, :]
    sin = np.sin(ang).astype(np.float32)
    cos = np.cos(ang).astype(np.float32)
    sign = np.where(np.arange(64) % 2 == 0, -1.0, 1.0).astype(np.float32)
    cosT = np.tile(cos.T, (2, 1))
    sinT = np.tile((sin * sign[None, :]).T, (2, 1))
    tab = {}
    tab["rotq"] = np.ascontiguousarray(np.stack([cosT[:, :TOWN], sinT[:, :TOWN]], 1)).astype(np.float32)
    tab["rotk"] = np.ascontiguousarray(np.stack([cosT * 0.125, sinT * 0.125], 1)).astype(np.float32)
    H = 4
    log_g = np.log(1.0 - 2.0 ** (-5.0 - np.arange(H, dtype=np.float64)))
    i = np.arange(128, dtype=np.float64)
    zeta = np.exp((127.0 - i)[:, None] * log_g[None, :])
    xi = np.exp((i + 1.0)[:, None] * log_g[None, :])
    mprime = np.zeros((128, H, 128), np.float64)
    jj = i[:, None]
    ii = i[None, :]
    for h in range(H):
        mprime[:, h, :] = np.where(jj <= ii, np.exp(-(jj + 1.0) * log_g[h]), 0.0)
    tab["zx"] = np.concatenate([zeta, xi], 1).astype(np.float32)
    tab["mprime"] = mprime.astype(np.float32)
    g = np.exp(128.0 * log_g)
    coef = np.zeros((128, 2, 9), np.float64)
    for jp in range(2):
        for p in range(128):
            gh = g[2 * jp + p // 64]
            if half == 0:
                c = [1, 0, gh ** 3, gh ** 2, gh, 1, gh, 1, 0]
            else:
                c = [gh, 1, gh ** 2, 1, gh, 0, gh ** 2, gh, 1]
            coef[p, jp, :] = c
    tab["coef"] = coef.astype(np.float32)
    tab["ident"] = np.eye(128, dtype=np.float32)
    k = 1
    qpos = np.concatenate([np.arange(own[2 * k] * 128, own[2 * k] * 128 + 128),
                           np.arange(own[2 * k + 1] * 128, own[2 * k + 1] * 128 + 128)])
    slots = [own[2 * k - 1], own[2 * k], own[2 * k + 1], oth[2 * k], oth[2 * k + 1]]
    rels = []
    for s in slots:
        kpos = np.arange(s * 128, s * 128 + 128)
        rels.append(qpos[None, :] - kpos[:, None])
    rel = np.stack(rels, 1)
    tab["bias_rel"] = rel
    tab["bias_bucket"] = t5_bucket_np(rel)
    return tab, own, oth


def check_structure():
    for half in (0, 1):
        tab, own, oth = const_tables(half)
        for k in range(8):
            qpos = np.concatenate([np.arange(own[2 * k] * 128, own[2 * k] * 128 + 128),
                                   np.arange(own[2 * k + 1] * 128, own[2 * k + 1] * 128 + 128)])
            special = {}
            if k > 0:
                special[own[2 * k - 1]] = 0
            special[own[2 * k]] = 1
            special[own[2 * k + 1]] = 2
            special[oth[2 * k]] = 3
            special[oth[2 * k + 1]] = 4
            keys = [own[j] for j in range(2 * k + 2)] + [oth[j] for j in range(2 * k + 2)]
            for gb in range(NBLK):
                kpos = np.arange(gb * 128, gb * 128 + 128)
                rel = qpos[None, :] - kpos[:, None]
                if gb in special:
                    s = special[gb]
                    assert np.array_equal(rel, tab["bias_rel"][:, s, :]), (half, k, gb)
                    if s in (2, 4):
                        assert (rel[:, :128] < 0).all()
                elif gb in keys:
                    assert (rel >= 113).all(), (half, k, gb)
                else:
                    assert (rel < 0).all(), (half, k, gb)
    return True


def prep_core(core, inp, tabs):
    b, half = core // 2, core % 2
    tab, own, oth = tabs[half]
    perm = own + oth
    x = inp["x"][b].reshape(NBLK, 128, D)
    m = {}
    m["xp"] = np.ascontiguousarray(x[perm].reshape(SEQ, D))
    m["cT"] = np.ascontiguousarray(inp["c"][b].reshape(8, 128).T)
    m["w_ada"] = inp["w_ada"][0]
    m["b_ada"] = inp["b_ada"][0].reshape(1, 6 * D)
    m["n12T"] = np.ascontiguousarray(np.concatenate([inp["norm1_w"][0].reshape(8, 128).T,
                                                     inp["norm2_w"][0].reshape(8, 128).T], 1))
    m["normfB"] = np.ascontiguousarray(np.broadcast_to(inp["normf_w"][None, :], (128, D)))
    w_in = inp["w_in"][0]
    m["w_in"] = w_in
    sw = w_in[:, :512].reshape(D, 256, 2)[:, :, ::-1].reshape(D, 512)
    m["w_sw"] = np.ascontiguousarray(sw)
    lam = np.concatenate([inp["lam_q1"][0], inp["lam_k1"][0], inp["lam_q2"][0], inp["lam_k2"][0]])
    m["lamB"] = np.ascontiguousarray(np.broadcast_to(lam[None, :], (128, 256)))
    m["sublnB"] = np.ascontiguousarray(np.broadcast_to(inp["subln_w"][0][None, :], (128, 128)))
    rb = inp["rel_bias"]
    bias = rb[tab["bias_bucket"]]
    bias = np.where(tab["bias_rel"][..., None] >= 0, bias, np.float32(NEG)).astype(np.float32)
    m["biasT"] = np.ascontiguousarray(np.transpose(bias, (0, 3, 1, 2)))
    m["cfar"] = np.ascontiguousarray(np.broadcast_to(rb[31][None, :], (128, 4)))
    m["w_out"] = inp["w_out"][0]
    m["wrT"] = np.ascontiguousarray(inp["w_router"][0].reshape(8, 128, NE).transpose(1, 0, 2))
    m["brB"] = np.ascontiguousarray(np.broadcast_to(inp["b_router"][0][None, :], (128, NE)))
    m["w1"] = inp["w1"][0]
    m["w2"] = inp["w2"][0]
    b1 = inp["b1"][0].reshape(NE, 8, 128, 2)
    m["b1T"] = np.ascontiguousarray(b1.transpose(2, 0, 1, 3))
    m["b2"] = inp["b2"][0]
    for k in ("rotq", "rotk", "zx", "mprime", "coef", "ident"):
        m[k] = tab[k]
    return m


def build_program(debug=False, n_experts=NE, n_diff=4, n_ret=2, stop=None):
    nc = bass.Bass("TRN2", target_bir_lowering=False)
    S = Sched(nc)
    dbg_outs = {}

    def din(name, shape):
        return nc.dram_tensor(name, list(shape), F32, kind="ExternalInput").ap()

    xp = din("xp", [SEQ, D])
    cT_d = din("cT", [128, 8])
    w_ada = din("w_ada", [D, 6 * D])
    b_ada = din("b_ada", [1, 6 * D])
    n12T_d = din("n12T", [128, 16])
    normfB_d = din("normfB", [128, D])
    w_in = din("w_in", [D, 3072])
    w_sw = din("w_sw", [D, 512])
    lamB_d = din("lamB", [128, 256])
    sublnB_d = din("sublnB", [128, 128])
    biasT_d = din("biasT", [128, 4, 5, 256])
    cfar_d = din("cfar", [128, 4])
    w_out = din("w_out", [D, D])
    wrT_d = din("wrT", [128, 8, NE])
    brB_d = din("brB", [128, NE])
    w1 = din("w1", [n_experts, D, 2 * D])
    w2 = din("w2", [n_experts, D, D])
    b1T_d = din("b1T", [128, NE, 8, 2])
    b2_d = din("b2", [NE, D])
    rotq_d = din("rotq", [128, 2, TOWN])
    rotk_d = din("rotk", [128, 2, SEQ])
    zx_d = din("zx", [128, 8])
    mprime_d = din("mprime", [128, 4, 128])
    coef_d = din("coef", [128, 2, 9])
    ident_d = din("ident", [128, 128])
    out_d = nc.dram_tensor("out", [TOWN, D], F32, kind="ExternalOutput").ap()

    def dbg_out(name, shape):
        t = nc.dram_tensor("dbg_" + name, list(shape), F32, kind="ExternalOutput").ap()
        dbg_outs[name] = t
        return t

    with ExitStack() as st:
        def sb(name, shape, dt=F32):
            return st.enter_context(nc.sbuf_tensor("s_" + name, list(shape), dt))

        arena1 = sb("arena1", [128, 8 * SEQ], BF16)
        hT = arena1[:, :].rearrange("p (c t) -> p c t", c=8)
        xres = arena1[:, :].bitcast(F32).rearrange("p (t d) -> p t d", t=NOWN)
        yTf = sb("yTf", [128, 8 * TOWN], BF16)
        yT = yTf[:, :].rearrange("p (c t) -> p c t", c=8)
        actT = yT
        wada_buf = [yTf[:, i * 8192:(i + 1) * 8192].bitcast(F32).rearrange("p (c n) -> p c n", c=8) for i in range(2)]
        arena3 = sb("arena3", [128, 30720], BF16)
        KT = arena3[:, 0:4096]
        QT = arena3[:, 4096:6144]
        Vb = arena3[:, 6144:14592].rearrange("p (t c) -> p t c", t=32)
        Gs = arena3[:, 14592:18688].rearrange("p (t c) -> p t c", t=16)
        biasT = arena3[:, 18688:21248].bitcast(F32).rearrange("p (s q) -> p s q", s=5)
        wsl = [arena3[:, 21248 + i * 1024:21248 + (i + 1) * 1024].rearrange("p (c n) -> p c n", c=8) for i in range(4)]
        wsl.append(arena3[:, 25344:27392].rearrange("p (c n) -> p c n", c=8))
        PT = [arena3[:, 27392 + i * 1024:27392 + (i + 1) * 1024] for i in range(3)]
        xsb = arena3[:, 28928:29952]
        h2T = arena3[:, 0:16384].rearrange("p (c t) -> p c t", c=8)
        w2b = arena3[:, 16384:24576].rearrange("p (c n) -> p c n", c=8)
        w1c = [arena3[:, 24576 + i * 2048:24576 + (i + 1) * 2048].rearrange("p (c n) -> p c n", c=8) for i in range(3)]
        arena4 = sb("arena4", [128, 4096], F32)
        rot = arena4[:, 0:1024].rearrange("p (a t) -> p a t", a=2)
        mprime = arena4[:, 1024:1536].rearrange("p (h i) -> p h i", h=4)
        kvc = arena4[:, 1536:2048].rearrange("p (s e) -> p s e", s=4)
        ybufs = arena4[:, 2048:2560].rearrange("p (s e) -> p s e", s=4)
        t0b = arena4[:, 2560:2816].rearrange("p (s e) -> p s e", s=2)
        Rst = arena4[:, 2816:2944]
        Ro = [arena4[:, 2944:3072], arena4[:, 3072:3200]]
        Rb = [arena4[:, 3200:3264].bitcast(BF16), arena4[:, 3264:3328].bitcast(BF16)]
        SM = [arena4[:, 3328:3392].bitcast(BF16), arena4[:, 3392:3456].bitcast(BF16),
              arena4[:, 3840:3904].bitcast(BF16), arena4[:, 3904:3968].bitcast(BF16)]
        yo = [arena4[:, 3456:3520].bitcast(BF16), arena4[:, 3520:3584].bitcast(BF16)]
        Kz4 = arena4[:, 3584:3840].bitcast(BF16).rearrange("p (s c) -> p s c", s=4)
        b1T = arena4[:, 0:512].rearrange("p (e c g) -> p e c g", e=NE, c=8)
        b1l7s = arena4[:, 512:768].rearrange("p (e c) -> p e c", e=NE)
        b2s = arena4[0:NE, 768:1792]
        gates = arena4[:, 1792:2304].rearrange("p (t e) -> p t e", t=NOWN)
        wrT = arena4[:, 2304:2560].rearrange("p (c e) -> p c e", c=8)
        gT = arena4[0:NE, 2560:2688]
        logit = arena4[:, 2688:2720]
        top8 = arena4[:, 2720:2728]
        rtmp = arena4[:, 2728:2824].rearrange("p (a e) -> p a e", a=3)
        brB = arena4[:, 2824:2856]
        gate12B = sb("gate12B", [128, 2, D])
        xin = [sb(f"xin{i}", [128, D]) for i in range(2)]
        tmpA = sb("tmpA", [128, D])
        tmpB = sb("tmpB", [128, D])
        tmpC = sb("tmpC", [128, D])
        junk = sb("junk", [128, D], BF16)
        identf = sb("identf", [128, 128])
        identb = sb("identb", [128, 128], BF16)
        small = sb("small", [128, 256])
        sublnB = sb("sublnB", [128, 128])
        cfar = sb("cfar", [128, 4])
        zx = sb("zx", [128, 8])
        coef = sb("coef", [128, 2, 9])
        psall = st.enter_context(nc.psum_tensor("psall", [128, 4096], F32))
        ps = [psall[:, i * 512:(i + 1) * 512] for i in range(8)]

        def ps_pair(b0):
            return psall[:, b0 * 512:(b0 + 2) * 512].rearrange("p (m q) -> p m q", m=2)[:, :, 0:256]

        _o = [0]

        def sc(n=1):
            v = small[:, _o[0]:_o[0] + n]
            _o[0] += n
            return v

        condT = sc(8)
        cTs = sc(8)
        n12T = sc(16)
        modT = sc(32)
        A1T = sc(8)
        A2T = sc(8)
        ssq = sc(32)
        sdv = sc(32)
        rstd = sc(32)
        epsc = sc(1)
        lamv = sc(4)
        neglam = sc(1)
        est = sc(16)
        onesr = sc(1)

        def PK(b):
            return ("ps", b)

        def dma(q, sem, out, in_, reads=(), writes=()):
            return S.dma(q, sem, lambda e, o_=out, i_=in_: e.dma_start(out=o_, in_=i_), reads=reads, writes=writes)

        def ld(name, dst, src):
            dma("sp", "c_" + name, dst, src, writes=[name])

        ld("identf", identf[:], ident_d[:, :])
        ld("cTs", cTs, cT_d[:, :])
        ld("n12T", n12T, n12T_d[:, :])
        ld("tmpA", tmpA[:, 0:256], lamB_d[:, :])
        ld("sublnB", sublnB[:], sublnB_d[:, :])
        ld("cfar", cfar[:], cfar_d[:, :])
        ld("zx", zx[:], zx_d[:, :])
        ld("mprime", mprime, mprime_d[:, :, :])
        ld("coef", coef[:], coef_d[:, :, :])
        S.op("dve", lambda e: e.tensor_copy(out=identb[:], in_=identf[:]), reads=["identf"], writes=["identb"])
        S.op("dve", lambda e: e.memset(epsc, EPS), writes=["epsc"])
        S.op("dve", lambda e: e.memset(ssq, 0.0), writes=[("ssq", t) for t in range(32)])
        S.op("dve", lambda e: e.tensor_scalar(out=sublnB[:], in0=sublnB[:], scalar1=1.0 - LAMBDA_INIT, scalar2=None, op0=ALU.mult),
             reads=["sublnB"], writes=["sublnB"])
        S.op("dve", lambda e: e.tensor_tensor(out=tmpB[:, 0:64], in0=tmpA[:, 0:64], in1=tmpA[:, 64:128], op=ALU.mult),
             reads=["tmpA"], writes=["tmpB"])
        S.op("dve", lambda e: e.tensor_tensor(out=tmpB[:, 64:128], in0=tmpA[:, 128:192], in1=tmpA[:, 192:256], op=ALU.mult),
             reads=["tmpA"], writes=["tmpB"])
        S.op("dve", lambda e: e.tensor_reduce(out=lamv[:, 0:2], in_=tmpB[:, 0:128].rearrange("p (a b) -> p a b", a=2),
                                              axis=AX.X, op=ALU.add), reads=["tmpB"], writes=["lamv"])
        S.op("act", lambda e: e.activation(out=lamv[:, 2:4], in_=lamv[:, 0:2], func=AF.Exp), reads=["lamv"], writes=["lamv"])
        S.op("dve", lambda e: e.tensor_tensor(out=neglam, in0=lamv[:, 3:4], in1=lamv[:, 2:3], op=ALU.subtract),
             reads=["lamv"], writes=["neglam"])
        S.op("dve", lambda e: e.tensor_scalar(out=neglam, in0=neglam, scalar1=-LAMBDA_INIT, scalar2=None, op0=ALU.add),
             reads=["neglam"], writes=["neglam"])

        S.op("act", lambda e: e.activation(out=condT, in_=cTs, func=AF.Silu), reads=["cTs"], writes=["condT"])
        S.op("dve", lambda e: e.memset(tmpB[0:1, 256:384], 1.0), writes=["tmpB"])
        ones_row = tmpB[0:1, 256:384]
        modrow = tmpC[0:1, 0:512]
        badar = tmpC[0:1, 512:1024]
        for fb in range(12):
            mi, half_ = fb // 2, fb % 2
            buf = wada_buf[fb % 2]
            for hh in range(2):
                dma("sp", f"wada{fb % 2}", buf[:, hh * 4:(hh + 1) * 4, :],
                    w_ada[hh * 512:(hh + 1) * 512, fb * 512:(fb + 1) * 512].rearrange("(c p) n -> p c n", p=128),
                    writes=[("wada", fb % 2)])
            dma("sp", "badar", badar, b_ada[0:1, fb * 512:(fb + 1) * 512], writes=["badar"])
            bank = ps[fb % 2]

            def mm(e, buf=buf, bank=bank):
                ins = None
                for kc in range(8):
                    ins = e.matmul(bank[0:1, :], lhsT=condT[:, kc:kc + 1], rhs=buf[:, kc, :], start=(kc == 0), stop=(kc == 7))
                return ins
            S.op("pe", mm, reads=["condT", ("wada", fb % 2)], writes=[PK(fb % 2)])
            S.op("dve", lambda e, bank=bank: e.tensor_tensor(out=modrow, in0=bank[0:1, :], in1=badar, op=ALU.add),
                 reads=[PK(fb % 2), "badar"], writes=["modrow"])
            if mi in (2, 5):
                gi = 0 if mi == 2 else 1
                S.op("pe", lambda e: e.matmul(ps[2][:, :], lhsT=ones_row, rhs=modrow, start=True, stop=True),
                     reads=["tmpB", "modrow"], writes=[PK(2)])
                S.op("act", lambda e, gi=gi, half_=half_: e.copy(out=gate12B[:, gi, half_ * 512:(half_ + 1) * 512], in_=ps[2][:, :]),
                     reads=[PK(2)], writes=["gate12B"])
            else:
                j = (0, 1, None, 2, 3)[mi]

                def mmT(e, j=j, half_=half_):
                    ins = None
                    for c4 in range(4):
                        col = j * 8 + half_ * 4 + c4
                        ins = e.matmul(ps[3][:, col:col + 1], lhsT=modrow[0:1, c4 * 128:(c4 + 1) * 128], rhs=ones_row[0:1, 0:1], start=True, stop=True)
                    return ins
                S.op("pe", mmT, reads=["modrow", "tmpB"], writes=[PK(3)])
                S.op("dve", lambda e, j=j, half_=half_: e.tensor_copy(out=modT[:, j * 8 + half_ * 4:j * 8 + half_ * 4 + 4],
                                                                      in_=ps[3][:, j * 8 + half_ * 4:j * 8 + half_ * 4 + 4]),
                     reads=[PK(3)], writes=["modT"])
        S.op("dve", lambda e: e.scalar_tensor_tensor(out=A1T, in0=modT[:, 8:16], scalar=1.0, in1=n12T[:, 0:8], op0=ALU.add, op1=ALU.mult),
             reads=["modT", "n12T"], writes=["A1T"])
        S.op("dve", lambda e: e.scalar_tensor_tensor(out=A2T, in0=modT[:, 24:32], scalar=1.0, in1=n12T[:, 8:16], op0=ALU.add, op1=ALU.mult),
             reads=["modT", "n12T"], writes=["A2T"])
        shift1T = modT[:, 0:8]
        shift2T = modT[:, 16:24]

        def bc(v, n):
            return v.unsqueeze(2).to_broadcast([128, n, 128])

        def hkey(c, tg):
            return ("hT", c, tg)

        if stop == 'p0':
            S.fence(); S.emit(); return nc, dbg_outs
        for t in range(NBLK):
            xb_ = xin[t % 2]
            dma("sp", f"xin{t % 2}", xb_[:], xp[t * 128:(t + 1) * 128, :], writes=[("xin", t % 2)])
            S.op("act", lambda e, xb_=xb_, t=t: e.activation(out=junk[:], in_=xb_[:], func=AF.Square, accum_out=ssq[:, t:t + 1]),
                 reads=[("xin", t % 2), ("ssq", t)], writes=["junk", ("ssq", t)])
            S.op("act", lambda e, t=t: e.activation(out=sdv[:, t:t + 1], in_=ssq[:, t:t + 1], func=AF.Sqrt, scale=1.0 / D, bias=epsc),
                 reads=[("ssq", t), "epsc"], writes=[("sdv", t)])
            S.op("dve", lambda e, t=t: e.reciprocal(out=rstd[:, t:t + 1], in_=sdv[:, t:t + 1]), reads=[("sdv", t)], writes=[("rstd", t)])
            S.op("act", lambda e, xb_=xb_, t=t: e.activation(out=xsb, in_=xb_[:], func=AF.Copy, scale=rstd[:, t:t + 1]),
                 reads=[("xin", t % 2), ("rstd", t)], writes=["xsb"])
            bi = 4 + t % 2
            bv = ps[bi][:, :].bitcast(BF16).rearrange("p (c t) -> p c t", c=8)

            def tr(e, bv=bv):
                ins = None
                for c in range(8):
                    ins = e.transpose(out=bv[:, c, :], in_=xsb[:, c * 128:(c + 1) * 128], identity=identb[:])
                return ins
            S.op("pe", tr, reads=["xsb", "identb"], writes=[PK(bi)])
            tv = tmpA[:, :].rearrange("p (c t) -> p c t", c=8)
            S.op("dve", lambda e, bv=bv, tv=tv: e.tensor_tensor(out=tv, in0=bv, in1=bc(A1T, 8), op=ALU.mult),
                 reads=[PK(bi), "A1T"], writes=["tmpA"])
            S.op("dve", lambda e, tv=tv, t=t: e.tensor_tensor(out=hT[:, :, t * 128:(t + 1) * 128], in0=tv, in1=bc(shift1T, 8), op=ALU.add),
                 reads=["tmpA", "modT"], writes=[hkey(c, t // 4) for c in range(8)])

        if debug:
            d = dbg_out("hT", [128, 8, SEQ])
            for c in range(8):
                for q4 in range(4):
                    S.op("dve", lambda e, c=c, q4=q4: e.tensor_copy(out=tmpA[:], in_=hT[:, c, q4 * 1024:(q4 + 1) * 1024]),
                         reads=[hkey(c, tg) for tg in range(8)], writes=["tmpA"])
                    dma("sp", "dbg", d[:, c, q4 * 1024:(q4 + 1) * 1024], tmpA[:], reads=["tmpA"], writes=["dbgout"])

        if stop == 'p1':
            S.fence(); S.emit(); return nc, dbg_outs
        def load_w(slot, src_ap, ncols):
            dma("pool", f"wsl{slot}", wsl[slot][:, :, 0:ncols], src_ap.rearrange("(c p) n -> p c n", p=128), writes=[("wsl", slot)])

        def proj_fm(slot, tg, bank_i):
            bank = ps[bank_i]

            def mm(e):
                ins = None
                for kc in range(8):
                    ins = e.matmul(bank[:, :], lhsT=wsl[slot][:, kc, 0:128], rhs=hT[:, kc, tg * 512:(tg + 1) * 512],
                                   start=(kc == 0), stop=(kc == 7))
                return ins
            S.op("pe", mm, reads=[("wsl", slot)] + [hkey(c, tg) for c in range(8)], writes=[PK(bank_i)])

        def proj_tm(slot, ncols, t0_, ntiles, bank_i):
            bank = ps[bank_i]

            def mm(e):
                ins = None
                for j in range(ntiles):
                    t = t0_ + j
                    for kc in range(8):
                        ins = e.matmul(bank[:, j * ncols:(j + 1) * ncols], lhsT=hT[:, kc, t * 128:(t + 1) * 128], rhs=wsl[slot][:, kc, 0:ncols],
                                       start=(kc == 0), stop=(kc == 7))
                return ins
            S.op("pe", mm, reads=[("wsl", slot)] + [hkey(c, tg) for c in range(8) for tg in sorted({(t0_ + j) // 4 for j in range(ntiles)})],
                 writes=[PK(bank_i)])

        def rotary_proj(slot_a, slot_b, tg, table_d, dst, dst_key):
            dma("sp", "rot", rot[:, :, :], table_d[:, :, tg * 512:(tg + 1) * 512], writes=["rot"])
            proj_fm(slot_a, tg, 0)
            proj_fm(slot_b, tg, 1)
            S.op("dve", lambda e: e.tensor_tensor(out=tmpA[:, 0:512], in0=ps[0][:, :], in1=rot[:, 0, :], op=ALU.mult),
                 reads=[PK(0), "rot"], writes=["tmpA"])
            S.op("dve", lambda e: e.tensor_tensor(out=tmpB[:, 0:512], in0=ps[1][:, :], in1=rot[:, 1, :], op=ALU.mult),
                 reads=[PK(1), "rot"], writes=["tmpB"])
            S.op("dve", lambda e: e.tensor_tensor(out=dst[:, tg * 512:(tg + 1) * 512], in0=tmpA[:, 0:512], in1=tmpB[:, 0:512], op=ALU.add),
                 reads=["tmpA", "tmpB"], writes=[(dst_key, tg)])

        ep_cnt = [0]

        def rms_epilogue(y_ap, mult_ap, chunk, tok_lo, ykey, mkey):
            if len(pending) >= 2:
                flush_pending()
            i = ep_cnt[0] % 2
            ep_cnt[0] += 1
            e0 = est[:, 4 * i:4 * i + 1]
            e1 = est[:, 4 * i + 1:4 * i + 2]
            e2 = est[:, 4 * i + 2:4 * i + 3]
            ek = ("est", i)
            S.op("dve", lambda e: e.memset(e0, 0.0), writes=[ek])
            S.op("act", lambda e: e.activation(out=junk[:, 0:128], in_=y_ap, func=AF.Square, accum_out=e0),
                 reads=[ykey, ek], writes=["junk", ek])
            S.op("act", lambda e: e.activation(out=e1, in_=e0, func=AF.Sqrt, scale=1.0 / 128, bias=epsc), reads=[ek, "epsc"], writes=[ek])
            S.op("dve", lambda e: e.reciprocal(out=e2, in_=e1), reads=[ek], writes=[ek])
            S.op("dve", lambda e: e.scalar_tensor_tensor(out=yo[i], in0=y_ap, scalar=e2, in1=mult_ap, op0=ALU.mult, op1=ALU.mult),
                 reads=[ykey, ek, mkey], writes=[("yo", i)])
            bv = ps[i][:, :].bitcast(BF16)

            def part_b():
                S.op("pe", lambda e: e.transpose(out=bv[:, 0:128], in_=yo[i], identity=identb[:]),
                     reads=[("yo", i), "identb"], writes=[PK(i)])
                S.op("act", lambda e: e.copy(out=yT[:, chunk, tok_lo:tok_lo + 128], in_=bv[:, 0:128]),
                     reads=[PK(i)], writes=[("yT", chunk, tok_lo // 128)])
            pending.append(part_b)

        pending = []

        def flush_pending():
            while pending:
                pending.pop(0)()

        def ret_group(jp, k):
            cf = [coef[:, jp, i:i + 1] for i in range(9)]
            blocks = [2 * k, 2 * k + 1, 16 + 2 * k, 17 + 2 * k]
            bv = ps[4][:, :].bitcast(BF16)

            def trk(e):
                ins = None
                for si, n in enumerate(blocks):
                    ins = e.transpose(out=bv[:, si * 128:(si + 1) * 128], in_=KT[:, n * 128:(n + 1) * 128], identity=identb[:])
                return ins
            S.op("pe", trk, reads=[("KT", n // 4) for n in blocks] + ["identb"], writes=[PK(4)])
            for hh in range(2):
                h = 2 * jp + hh
                S.op("act", lambda e, hh=hh, h=h: e.activation(out=Kz4[:, :, hh * 64:(hh + 1) * 64],
                                                               in_=bv[:, 0:512].rearrange("p (s c) -> p s c", s=4)[:, :, hh * 64:(hh + 1) * 64],
                                                               func=AF.Copy, scale=zx[:, h:h + 1]),
                     reads=[PK(4), "zx"], writes=[("Kz4", hh)])
            for si, n in enumerate(blocks):
                bi = 5 + si // 2
                off = (si % 2) * 256

                def mmkv(e, n=n, bi=bi, off=off, si=si):
                    e.matmul(ps[bi][:, off:off + 128], lhsT=Kz4[:, si, :], rhs=Vb[:, n, 0:128], start=True, stop=True)
                    return e.matmul(ps[bi][:, off + 128:off + 256], lhsT=Kz4[:, si, :], rhs=Vb[:, n, 128:256], start=True, stop=True)
                S.op("pe", mmkv, reads=[("Kz4", 0), ("Kz4", 1), ("V", n)], writes=[PK(bi)])
                S.op("act", lambda e, si=si, bi=bi, off=off: e.copy(out=kvc[0:64, si, :], in_=ps[bi][0:64, off:off + 128]),
                     reads=[PK(bi)], writes=[("kvc", si)])
                S.op("act", lambda e, si=si, bi=bi, off=off: e.copy(out=kvc[64:128, si, :], in_=ps[bi][64:128, off + 128:off + 256]),
                     reads=[PK(bi)], writes=[("kvc", si)])
            T = [tmpC[:, i * 128:(i + 1) * 128] for i in range(6)]
            S.op("dve", lambda e: e.tensor_scalar(out=T[0], in0=kvc[:, 2, :], scalar1=cf[1], scalar2=None, op0=ALU.mult),
                 reads=[("kvc", 2), "coef"], writes=["tmpC"])
            S.op("dve", lambda e: e.scalar_tensor_tensor(out=Ro[0], in0=Rst, scalar=cf[0], in1=T[0], op0=ALU.mult, op1=ALU.add),
                 reads=["Rst", "tmpC", "coef"], writes=[("Ro", 0)])
            S.op("dve", lambda e: e.tensor_scalar(out=T[1], in0=kvc[:, 0, :], scalar1=cf[3], scalar2=None, op0=ALU.mult),
                 reads=[("kvc", 0), "coef"], writes=["tmpC"])
            S.op("dve", lambda e: e.scalar_tensor_tensor(out=T[2], in0=kvc[:, 2, :], scalar=cf[4], in1=T[1], op0=ALU.mult, op1=ALU.add),
                 reads=[("kvc", 2), "tmpC", "coef"], writes=["tmpC"])
            S.op("dve", lambda e: e.scalar_tensor_tensor(out=T[3], in0=kvc[:, 3, :], scalar=cf[5], in1=T[2], op0=ALU.mult, op1=ALU.add),
                 reads=[("kvc", 3), "tmpC", "coef"], writes=["tmpC"])
            S.op("dve", lambda e: e.scalar_tensor_tensor(out=Ro[1], in0=Rst, scalar=cf[2], in1=T[3], op0=ALU.mult, op1=ALU.add),
                 reads=["Rst", "tmpC", "coef"], writes=[("Ro", 1)])
            S.op("dve", lambda e: e.tensor_scalar(out=T[4], in0=kvc[:, 1, :], scalar1=cf[7], scalar2=None, op0=ALU.mult),
                 reads=[("kvc", 1), "coef"], writes=["tmpC"])
            S.op("dve", lambda e: e.scalar_tensor_tensor(out=T[5], in0=kvc[:, 3, :], scalar=cf[8], in1=T[4], op0=ALU.mult, op1=ALU.add),
                 reads=[("kvc", 3), "tmpC", "coef"], writes=["tmpC"])
            S.op("dve", lambda e: e.scalar_tensor_tensor(out=Rst, in0=Ro[1], scalar=cf[6], in1=T[5], op0=ALU.mult, op1=ALU.add),
                 reads=[("Ro", 1), "tmpC", "coef"], writes=["Rst"])
            for qi in range(2):
                S.op("act", lambda e, qi=qi: e.copy(out=Rb[qi], in_=Ro[qi]), reads=[("Ro", qi)], writes=[("Rb", qi)])
            combos = [(qi, hh) for qi in range(2) for hh in range(2)]
            for qi, hh in combos:
                i = 2 * k + qi
                pr = slice(hh * 64, (hh + 1) * 64)
                S.op("pe", lambda e, hh=hh, pr=pr, i=i, qi=qi: e.matmul(ps[2 + hh][:, qi * 256:qi * 256 + 128], lhsT=KT[pr, i * 128:(i + 1) * 128],
                                                                    rhs=QT[pr, i * 128:(i + 1) * 128], start=True, stop=True),
                     reads=[("KT", i // 4), ("QT", i // 4)], writes=[PK(2 + hh)])
            flush_pending()
            for qi, hh in combos:
                h = 2 * jp + hh
                ci = 2 * qi + hh
                S.op("dve", lambda e, hh=hh, h=h, qi=qi, ci=ci: e.tensor_tensor(out=SM[ci], in0=ps[2 + hh][:, qi * 256:qi * 256 + 128], in1=mprime[:, h, :], op=ALU.mult),
                     reads=[PK(2 + hh), "mprime"], writes=[("SM", ci)])
            for qi, hh in combos:
                i = 2 * k + qi
                ci = 2 * qi + hh
                pr = slice(hh * 64, (hh + 1) * 64)

                def mmacc(e, hh=hh, pr=pr, i=i, qi=qi, ci=ci):
                    o = qi * 256 + 128
                    e.matmul(ps[2 + hh][:, o:o + 128], lhsT=QT[pr, i * 128:(i + 1) * 128], rhs=Rb[qi][pr, :], start=True, stop=False)
                    return e.matmul(ps[2 + hh][:, o:o + 128], lhsT=SM[ci], rhs=Vb[:, i, hh * 128:(hh + 1) * 128], start=False, stop=True)
                S.op("pe", mmacc, reads=[("QT", i // 4), ("Rb", qi), ("SM", ci), ("V", i)], writes=[PK(2 + hh)])
            for qi, hh in combos:
                i = 2 * k + qi
                h = 2 * jp + hh
                ci = 2 * qi + hh
                yb = ybufs[:, ci, :]
                yk = ("ybuf", ci)
                S.op("dve", lambda e, hh=hh, yb=yb, h=h, qi=qi: e.tensor_scalar(out=yb, in0=ps[2 + hh][:, qi * 256 + 128:qi * 256 + 256], scalar1=zx[:, 4 + h:5 + h],
                                                                               scalar2=None, op0=ALU.mult),
                     reads=[PK(2 + hh), "zx"], writes=[yk])
                rms_epilogue(yb, Gs[:, i, hh * 128:(hh + 1) * 128], h, i * 128, yk, "Gs")

        for jp in range(n_ret):
            load_w(0, w_in[:, jp * 128:(jp + 1) * 128], 128)
            load_w(1, w_sw[:, jp * 128:(jp + 1) * 128], 128)
            load_w(2, w_in[:, 256 + jp * 128:256 + (jp + 1) * 128], 128)
            load_w(3, w_sw[:, 256 + jp * 128:256 + (jp + 1) * 128], 128)
            load_w(4, w_in[:, 512 + jp * 256:512 + (jp + 1) * 256], 256)
            for tg in range(8):
                rotary_proj(2, 3, tg, rotk_d, KT, "KT")
            for tg in range(4):
                rotary_proj(0, 1, tg, rotq_d, QT, "QT")
            for t2 in range(NBLK // 2):
                bi = 2 + t2 % 2
                proj_tm(4, 256, 2 * t2, 2, bi)
                S.op("act", lambda e, t2=t2, bi=bi: e.copy(out=Vb[:, 2 * t2:2 * t2 + 2, 0:256], in_=ps[bi][:, :].rearrange("p (j c) -> p j c", j=2)),
                     reads=[PK(bi)], writes=[("V", 2 * t2), ("V", 2 * t2 + 1)])
            load_w(4, w_in[:, 1024 + jp * 256:1024 + (jp + 1) * 256], 256)
            for t2 in range(NOWN // 2):
                bi = 2 + t2 % 2
                proj_tm(4, 256, 2 * t2, 2, bi)
                S.op("act", lambda e, t2=t2, bi=bi: e.activation(out=Gs[:, 2 * t2:2 * t2 + 2, :], in_=ps[bi][:, :].rearrange("p (j c) -> p j c", j=2), func=AF.Silu),
                     reads=[PK(bi)], writes=["Gs"])
            S.op("dve", lambda e: e.memset(Rst, 0.0), writes=["Rst"])
            for k in range(8):
                ret_group(jp, k)
            flush_pending()

        def dump_yT():
            d = dbg_out("yT", [128, 8, TOWN])
            for c in range(8):
                for q2 in range(2):
                    S.op("dve", lambda e, c=c, q2=q2: e.tensor_copy(out=tmpA[:], in_=yT[:, c, q2 * 1024:(q2 + 1) * 1024]),
                         reads=[("yT", c, t) for t in range(NOWN)], writes=["tmpA"])
                    dma("sp", "dbg", d[:, c, q2 * 1024:(q2 + 1) * 1024], tmpA[:], reads=["tmpA"], writes=["dbgout"])

        if stop == 'ret':
            if debug:
                dump_yT()
            S.fence(); S.emit(); return nc, dbg_outs
        def diff_group(h, k):
            far = [j for j in range(2 * k - 1)] + [16 + j for j in range(2 * k)]
            spec = ([(2 * k - 1, 0)] if k > 0 else []) + [(2 * k, 1), (16 + 2 * k, 3), (2 * k + 1, 2), (17 + 2 * k, 4)]
            steps = [(kb, None) for kb in far] + spec
            nst = len(steps)
            assert nst % 2 == 0
            npair = nst // 2
            last_q0 = max(i for i, (kb, s) in enumerate(steps) if s not in (2, 4))

            def stage_s(pi):
                b0 = 2 + 2 * (pi % 2)
                reg = psall[:, b0 * 512:(b0 + 2) * 512].rearrange("p (m j q) -> p m j q", m=2, j=2)

                def mm(e):
                    ins = None
                    for j in range(2):
                        kb = steps[2 * pi + j][0]
                        for m in range(2):
                            pr = slice(m * 64, (m + 1) * 64)
                            ins = e.matmul(ps[b0 + m][:, j * 256:(j + 1) * 256], lhsT=KT[pr, kb * 128:(kb + 1) * 128],
                                           rhs=QT[pr, k * 256:(k + 1) * 256], start=True, stop=True)
                    return ins
                S.op("pe", mm, reads=[("KT", steps[2 * pi + j][0] // 4) for j in range(2)] + [("QT", k // 2)], writes=[PK(b0), PK(b0 + 1)])
                pt = PT[pi % 3]
                ptk = ("PT", pi % 3)
                if steps[2 * pi][1] is None and steps[2 * pi + 1][1] is None:
                    S.op("act", lambda e: e.activation(out=pt.rearrange("p (j m q) -> p m j q", j=2, m=2), in_=reg, func=AF.Exp,
                                                       scale=0.125, bias=cfar[:, h:h + 1]),
                         reads=[PK(b0), PK(b0 + 1), "cfar"], writes=[ptk])
                else:
                    for j in range(2):
                        s = steps[2 * pi + j][1]
                        tv = tmpA[:, j * 512:(j + 1) * 512].rearrange("p (m q) -> p m q", m=2)
                        src = reg[:, :, j, :]
                        if s is None:
                            S.op("dve", lambda e, tv=tv, src=src: e.tensor_scalar(out=tv, in0=src, scalar1=0.125, scalar2=cfar[:, h:h + 1],
                                                                                  op0=ALU.mult, op1=ALU.add),
                                 reads=[PK(b0), PK(b0 + 1), "cfar"], writes=["tmpA"])
                        else:
                            S.op("dve", lambda e, tv=tv, src=src, s=s: e.scalar_tensor_tensor(out=tv, in0=src, scalar=0.125,
                                                                                            in1=biasT[:, s, :].unsqueeze(1).to_broadcast([128, 2, 256]),
                                                                                            op0=ALU.mult, op1=ALU.add),
                                 reads=[PK(b0), PK(b0 + 1), "biasT"], writes=["tmpA"])
                    S.op("act", lambda e: e.activation(out=pt, in_=tmpA[:, :], func=AF.Exp), reads=["tmpA"], writes=[ptk])

            def stage_pv(pi):
                pt = PT[pi % 3]

                def mm(e):
                    ins = None
                    for j in range(2):
                        fi = 2 * pi + j
                        kb, s = steps[fi]
                        for m in range(2):
                            for qi in range(2):
                                if qi == 0 and s in (2, 4):
                                    continue
                                lastidx = last_q0 if qi == 0 else nst - 1
                                ins = e.matmul(ps[6 + m][:, qi * 129:(qi + 1) * 129],
                                               lhsT=pt[:, j * 512 + m * 256 + qi * 128:j * 512 + m * 256 + (qi + 1) * 128],
                                               rhs=Vb[:, kb, 0:129], start=(fi == 0 and qi == 0), stop=(fi == lastidx), skip_group_check=True)
                    return ins
                S.op("pe", mm, reads=[("PT", pi % 3)] + [("V", steps[2 * pi + j][0]) for j in range(2)], writes=[PK(6), PK(7)])

            stage_s(0)
            for pi in range(npair):
                if pi + 1 < npair:
                    stage_s(pi + 1)
                if pi == 0:
                    flush_pending()
                stage_pv(pi)
            for qi in range(2):
                i = 2 * k + qi
                ei = est[:, 8 + 3 * qi: 8 + 3 * qi + 3]
                ek = ("est2", qi)
                S.op("dve", lambda e, ei=ei, qi=qi: e.reciprocal(out=ei[:, 0:1], in_=ps[6][:, qi * 129 + 128:qi * 129 + 129]),
                     reads=[PK(6)], writes=[ek])
                S.op("dve", lambda e, ei=ei, qi=qi: e.reciprocal(out=ei[:, 1:2], in_=ps[7][:, qi * 129 + 128:qi * 129 + 129]),
                     reads=[PK(7)], writes=[ek])
                S.op("dve", lambda e, ei=ei: e.tensor_tensor(out=ei[:, 2:3], in0=ei[:, 1:2], in1=neglam, op=ALU.mult),
                     reads=[ek, "neglam"], writes=[ek])
                t0 = t0b[:, qi, :]
                S.op("dve", lambda e, ei=ei, qi=qi, t0=t0: e.tensor_scalar(out=t0, in0=ps[6][:, qi * 129:qi * 129 + 128], scalar1=ei[:, 0:1], scalar2=None, op0=ALU.mult),
                     reads=[PK(6), ek], writes=[("t0", qi)])
                yb = ybufs[:, qi, :]
                yk = ("ybuf", qi)
                S.op("dve", lambda e, ei=ei, qi=qi, t0=t0, yb=yb: e.scalar_tensor_tensor(out=yb, in0=ps[7][:, qi * 129:qi * 129 + 128], scalar=ei[:, 2:3],
                                                                                       in1=t0, op0=ALU.mult, op1=ALU.add),
                     reads=[PK(7), ek, ("t0", qi)], writes=[yk])
                rms_epilogue(yb, sublnB[:], 4 + h, i * 128, yk, "sublnB")

        for h in range(n_diff):
            load_w(0, w_in[:, 1536 + h * 128:1536 + (h + 1) * 128], 128)
            load_w(2, w_in[:, 2048 + h * 128:2048 + (h + 1) * 128], 128)
            load_w(1, w_in[:, 2560 + h * 128:2560 + (h + 1) * 128], 128)
            dma("sp", "biasT", biasT, biasT_d[:, h, :, :], writes=["biasT"])
            for tg in range(8):
                proj_fm(2, tg, tg % 2)
                S.op("act", lambda e, tg=tg: e.copy(out=KT[:, tg * 512:(tg + 1) * 512], in_=ps[tg % 2][:, :]),
                     reads=[PK(tg % 2)], writes=[("KT", tg)])
            for tg in range(4):
                proj_fm(0, tg, tg % 2)
                S.op("act", lambda e, tg=tg: e.copy(out=QT[:, tg * 512:(tg + 1) * 512], in_=ps[tg % 2][:, :]),
                     reads=[PK(tg % 2)], writes=[("QT", tg)])
            for t4 in range(NBLK // 4):
                bi = 2 + t4 % 2
                proj_tm(1, 128, 4 * t4, 4, bi)
                S.op("act", lambda e, t4=t4, bi=bi: e.copy(out=Vb[:, 4 * t4:4 * t4 + 4, 0:128], in_=ps[bi][:, :].rearrange("p (j c) -> p j c", j=4)),
                     reads=[PK(bi)], writes=[("V", 4 * t4 + j) for j in range(4)])
            S.op("dve", lambda e: e.memset(Vb[:, :, 128:129], 1.0), writes=[("V", t) for t in range(NBLK)])
            for k in range(int(os.environ.get('DIFF_K', '8'))):
                diff_group(h, k)
            flush_pending()

        if debug:
            dump_yT()

        if stop == 'diff':
            S.fence(); S.emit(); return nc, dbg_outs
        S.fence()
        ld("wrT", wrT, wrT_d[:, :, :])
        ld("brB", brB, brB_d[:, :])
        ld("b1T", b1T, b1T_d[:, :, :, :])
        ld("b2s", b2s, b2_d[:, :])
        for hh in range(2):
            dma("pool", "w2b", w2b[:, hh * 4:(hh + 1) * 4, :], w_out[hh * 512:(hh + 1) * 512, :].rearrange("(c p) n -> p c n", p=128),
                writes=["w2b"])
        for t in range(NOWN):
            xb_ = xin[t % 2]
            dma("sp", f"xin{t % 2}", xb_[:], xp[t * 128:(t + 1) * 128, :], writes=[("xin", t % 2)])
            for nh in range(2):
                bank = ps[nh]

                def mm(e, bank=bank, nh=nh, t=t):
                    ins = None
                    for kc in range(8):
                        ins = e.matmul(bank[:, :], lhsT=yT[:, kc, t * 128:(t + 1) * 128], rhs=w2b[:, kc, nh * 512:(nh + 1) * 512],
                                       start=(kc == 0), stop=(kc == 7))
                    return ins
                S.op("pe", mm, reads=[("yT", c, t) for c in range(8)] + ["w2b"], writes=[PK(nh)])
                S.op("dve", lambda e, bank=bank, nh=nh: e.tensor_tensor(out=tmpA[:, nh * 512:(nh + 1) * 512], in0=bank[:, :],
                                                                       in1=gate12B[:, 0, nh * 512:(nh + 1) * 512], op=ALU.mult),
                     reads=[PK(nh), "gate12B"], writes=["tmpA"])
                S.op("dve", lambda e, xb_=xb_, nh=nh, t=t: e.tensor_tensor(out=xres[:, t, nh * 512:(nh + 1) * 512], in0=xb_[:, nh * 512:(nh + 1) * 512],
                                                                          in1=tmpA[:, nh * 512:(nh + 1) * 512], op=ALU.add),
                     reads=[("xin", t % 2), "tmpA"], writes=[("xres", t)])

        if debug:
            d = dbg_out("x1", [TOWN, D])
            for t in range(NOWN):
                dma("sp", "dbg", d[t * 128:(t + 1) * 128, :], xres[:, t, :], reads=[("xres", t)], writes=["dbgout"])

        def norm_stats(t, col):
            S.op("dve", lambda e: e.memset(ssq[:, col:col + 1], 0.0), writes=[("ssq", col)])
            S.op("act", lambda e: e.activation(out=junk[:], in_=xres[:, t, :], func=AF.Square, accum_out=ssq[:, col:col + 1]),
                 reads=[("xres", t), ("ssq", col)], writes=["junk", ("ssq", col)])
            S.op("act", lambda e: e.activation(out=sdv[:, col:col + 1], in_=ssq[:, col:col + 1], func=AF.Sqrt, scale=1.0 / D, bias=epsc),
                 reads=[("ssq", col), "epsc"], writes=[("sdv", col)])
            S.op("dve", lambda e: e.reciprocal(out=rstd[:, col:col + 1], in_=sdv[:, col:col + 1]), reads=[("sdv", col)], writes=[("rstd", col)])

        S.fence()
        h2f = tmpC[:, :].rearrange("p (c t) -> p c t", c=8)
        for t in range(NOWN):
            norm_stats(t, t)
            xs2 = xin[t % 2]
            S.op("act", lambda e, t=t, xs2=xs2: e.activation(out=xs2[:], in_=xres[:, t, :], func=AF.Copy, scale=rstd[:, t:t + 1]),
                 reads=[("xres", t), ("rstd", t)], writes=[("xin", t % 2)])

            def tr(e, xs2=xs2):
                ins = None
                for c in range(8):
                    ins = e.transpose(out=ps[2 + c // 4][:, (c % 4) * 128:(c % 4 + 1) * 128], in_=xs2[:, c * 128:(c + 1) * 128], identity=identf[:])
                return ins
            S.op("pe", tr, reads=[("xin", t % 2), "identf"], writes=[PK(2), PK(3)])
            for hh in range(2):
                S.op("dve", lambda e, hh=hh: e.tensor_tensor(out=tmpB[:, hh * 512:(hh + 1) * 512].rearrange("p (c t) -> p c t", c=4),
                                                            in0=ps[2 + hh][:, :].rearrange("p (c t) -> p c t", c=4),
                                                            in1=bc(A2T[:, hh * 4:(hh + 1) * 4], 4), op=ALU.mult),
                     reads=[PK(2 + hh), "A2T"], writes=["tmpB"])
                S.op("dve", lambda e, hh=hh: e.tensor_tensor(out=h2f[:, hh * 4:(hh + 1) * 4, :],
                                                            in0=tmpB[:, hh * 512:(hh + 1) * 512].rearrange("p (c t) -> p c t", c=4),
                                                            in1=bc(shift2T[:, hh * 4:(hh + 1) * 4], 4), op=ALU.add),
                     reads=["tmpB", "modT"], writes=["tmpC"])
            S.op("act", lambda e, t=t: e.copy(out=h2T[:, :, t * 128:(t + 1) * 128], in_=h2f), reads=["tmpC"], writes=[("h2T", t)])

            def mmr(e):
                ins = None
                for kc in range(8):
                    ins = e.matmul(ps[4][:, 0:NE], lhsT=h2f[:, kc, :], rhs=wrT[:, kc, :], start=(kc == 0), stop=(kc == 7))
                return ins
            S.op("pe", mmr, reads=["tmpC", "wrT"], writes=[PK(4)])
            S.op("dve", lambda e: e.tensor_tensor(out=logit, in0=ps[4][:, 0:NE], in1=brB, op=ALU.add),
                 reads=[PK(4), "brB"], writes=["logit"])
            S.op("dve", lambda e: e.max(out=top8, in_=logit), reads=["logit"], writes=["top8"])
            S.op("dve", lambda e: e.tensor_scalar(out=est[:, 0:1], in0=top8[:, 0:1], scalar1=-1.0, scalar2=None, op0=ALU.mult),
                 reads=["top8"], writes=[("est", 0)])
            S.op("act", lambda e: e.activation(out=rtmp[:, 0, :], in_=logit, func=AF.Exp, bias=est[:, 0:1]),
                 reads=["logit", ("est", 0)], writes=[("rtmp", 0)])
            S.op("dve", lambda e: e.tensor_scalar(out=rtmp[:, 1, :], in0=logit, scalar1=top8[:, 3:4], scalar2=None, op0=ALU.is_ge),
                 reads=["logit", "top8"], writes=[("rtmp", 1)])
            S.op("dve", lambda e: e.tensor_tensor(out=rtmp[:, 2, :], in0=rtmp[:, 0, :], in1=rtmp[:, 1, :], op=ALU.mult),
                 reads=[("rtmp", 0), ("rtmp", 1)], writes=[("rtmp", 2)])
            S.op("dve", lambda e: e.tensor_reduce(out=est[:, 1:2], in_=rtmp[:, 2, :], axis=AX.X, op=ALU.add),
                 reads=[("rtmp", 2)], writes=[("est", 0)])
            S.op("dve", lambda e: e.reciprocal(out=est[:, 2:3], in_=est[:, 1:2]), reads=[("est", 0)], writes=[("est", 0)])
            S.op("dve", lambda e, t=t: e.tensor_scalar(out=gates[:, t, :], in0=rtmp[:, 2, :], scalar1=est[:, 2:3], scalar2=None, op0=ALU.mult),
                 reads=[("rtmp", 2), ("est", 0)], writes=[("gates", t)])
            S.op("pe", lambda e, t=t: e.transpose(out=ps[5][0:NE, 0:128], in_=gates[:, t, :], identity=identf[:]),
                 reads=[("gates", t), "identf"], writes=[PK(5)])
            S.op("act", lambda e: e.copy(out=gT, in_=ps[5][0:NE, 0:128]), reads=[PK(5)], writes=["gT"])
            for nh in range(2):
                S.op("pe", lambda e, nh=nh: e.matmul(ps[nh][:, :], lhsT=gT, rhs=b2s[:, nh * 512:(nh + 1) * 512], start=True, stop=True),
                     reads=["gT", "b2s"], writes=[PK(nh)])
                S.op("dve", lambda e, nh=nh: e.tensor_tensor(out=tmpB[:, nh * 512:(nh + 1) * 512], in0=ps[nh][:, :],
                                                            in1=gate12B[:, 1, nh * 512:(nh + 1) * 512], op=ALU.mult),
                     reads=[PK(nh), "gate12B"], writes=["tmpB"])
                S.op("dve", lambda e, nh=nh, t=t: e.tensor_tensor(out=xres[:, t, nh * 512:(nh + 1) * 512], in0=xres[:, t, nh * 512:(nh + 1) * 512],
                                                                 in1=tmpB[:, nh * 512:(nh + 1) * 512], op=ALU.add),
                     reads=[("xres", t), "tmpB"], writes=[("xres", t)])

        if debug:
            d = dbg_out("gates", [128, NOWN * NE])
            dma("sp", "dbg", d[:, :], gates.rearrange("p t e -> p (t e)"), reads=[("gates", t) for t in range(NOWN)], writes=["dbgout"])

        if stop == 'p3':
            S.fence(); S.emit(); return nc, dbg_outs
        S.fence()
        S.op("dve", lambda e: e.tensor_scalar(out=b1l7s, in0=b1T[:, :, :, 1], scalar1=7.0, scalar2=1.0 / ALPHA, op0=ALU.add, op1=ALU.mult),
             reads=["b1T"], writes=["b1l7s"])
        gsb = [tmpA[:, 0:512], tmpA[:, 512:1024]]
        rlb = [tmpB[:, 0:512], tmpB[:, 512:1024]]
        tbb = [tmpC[:, 0:512], tmpC[:, 512:1024]]
        ucnt = 0
        wcnt = 0
        for ex in range(n_experts):
            for hh in range(2):
                dma("pool", "w2b", w2b[:, hh * 4:(hh + 1) * 4, :], w2[ex, hh * 512:(hh + 1) * 512, :].rearrange("(c p) n -> p c n", p=128),
                    writes=["w2b"])
            for fc in range(8):
                wb = w1c[wcnt % 3]
                wk = ("w1c", wcnt % 3)
                dma("pool", f"w1c{wcnt % 3}", wb[:, :, :], w1[ex, :, fc * 256:(fc + 1) * 256].rearrange("(c p) n -> p c n", p=128),
                    writes=[wk])
                wcnt += 1
                for tg in range(4):
                    u = ucnt % 4
                    v = ucnt % 2
                    ucnt += 1
                    for gl in range(2):
                        bank = ps[2 * u + gl]

                        def mm(e, bank=bank, wb=wb, gl=gl, tg=tg):
                            ins = None
                            for kc in range(8):
                                ins = e.matmul(bank[:, :], lhsT=wb[:, kc, gl::2], rhs=h2T[:, kc, tg * 512:(tg + 1) * 512],
                                               start=(kc == 0), stop=(kc == 7))
                            return ins
                        S.op("pe", mm, reads=[wk] + [("h2T", t) for t in range(tg * 4, tg * 4 + 4)], writes=[PK(2 * u + gl)])
                    gk, rk = ("gsb", v), ("rlb", v)
                    S.op("dve", lambda e, u=u, v=v, ex=ex, fc=fc: e.tensor_scalar(out=gsb[v], in0=ps[2 * u][:, :], scalar1=b1T[:, ex, fc, 0:1], scalar2=7.0,
                                                                             op0=ALU.add, op1=ALU.min),
                         reads=[PK(2 * u), "b1T"], writes=[gk])
                    S.op("act", lambda e, v=v: e.activation(out=gsb[v], in_=gsb[v], func=AF.Silu, scale=ALPHA),
                         reads=[gk], writes=[gk])
                    S.op("act", lambda e, u=u, v=v, ex=ex, fc=fc: e.activation(out=rlb[v], in_=ps[2 * u + 1][:, :], func=AF.Relu, scale=1.0 / ALPHA,
                                                                          bias=b1l7s[:, ex, fc:fc + 1]),
                         reads=[PK(2 * u + 1), "b1l7s"], writes=[rk])
                    S.op("dve", lambda e, v=v: e.tensor_scalar(out=rlb[v], in0=rlb[v], scalar1=14.0 / ALPHA, scalar2=-6.0 / ALPHA,
                                                               op0=ALU.min, op1=ALU.add),
                         reads=[rk], writes=[rk])
                    S.op("dve", lambda e, v=v, fc=fc, tg=tg: e.tensor_tensor(out=actT[:, fc, tg * 512:(tg + 1) * 512], in0=gsb[v], in1=rlb[v], op=ALU.mult),
                         reads=[gk, rk], writes=[("yT", fc, t) for t in range(tg * 4, tg * 4 + 4)])
            for t in range(NOWN):
                for nh in range(2):
                    bi = (2 * t + nh) % 8
                    bank = ps[bi]

                    def mm(e, bank=bank, t=t, nh=nh):
                        ins = None
                        for fc in range(8):
                            ins = e.matmul(bank[:, :], lhsT=actT[:, fc, t * 128:(t + 1) * 128], rhs=w2b[:, fc, nh * 512:(nh + 1) * 512],
                                           start=(fc == 0), stop=(fc == 7))
                        return ins
                    S.op("pe", mm, reads=[("yT", fc, t) for fc in range(8)] + ["w2b"], writes=[PK(bi)])
                    tb = tbb[(2 * t + nh) % 2]
                    tk = ("tbb", (2 * t + nh) % 2)
                    S.op("dve", lambda e, bank=bank, t=t, nh=nh, ex=ex, tb=tb: e.scalar_tensor_tensor(out=tb, in0=bank[:, :], scalar=gates[:, t, ex:ex + 1],
                                                                                                   in1=gate12B[:, 1, nh * 512:(nh + 1) * 512],
                                                                                                   op0=ALU.mult, op1=ALU.mult),
                         reads=[PK(bi), ("gates", t), "gate12B"], writes=[tk])
                    S.op("dve", lambda e, t=t, nh=nh, tb=tb: e.tensor_tensor(out=xres[:, t, nh * 512:(nh + 1) * 512], in0=xres[:, t, nh * 512:(nh + 1) * 512],
                                                                            in1=tb, op=ALU.add),
                         reads=[("xres", t), tk], writes=[("xres", t)])

        if stop == 'moe':
            S.fence(); S.emit(); return nc, dbg_outs
        S.fence()
        nfB = tmpA
        ld("nfB", nfB[:], normfB_d[:, :])
        for t in range(NOWN):
            norm_stats(t, 16 + t)
            ob = xin[t % 2]
            S.op("dve", lambda e, t=t, ob=ob: e.scalar_tensor_tensor(out=ob[:], in0=xres[:, t, :], scalar=rstd[:, 16 + t:17 + t], in1=nfB[:],
                                                                    op0=ALU.mult, op1=ALU.mult),
                 reads=[("xres", t), ("rstd", 16 + t), "nfB"], writes=[("xin", t % 2)])
            dma("sp", f"out{t % 2}", out_d[t * 128:(t + 1) * 128, :], ob[:], reads=[("xin", t % 2)], writes=[("out", t)])
        S.fence()
        S.emit()
    return nc, dbg_outs


_CACHE = {}


def kernel(**inputs):
    inp = {k: np.asarray(v) for k, v in inputs.items()}
    tabs = [const_tables(0), const_tables(1)]
    in_maps = [prep_core(c, inp, tabs) for c in range(8)]
    if "nc" not in _CACHE:
        _CACHE["nc"] = build_program()[0]
    nc = _CACHE["nc"]
    res = run_bass_kernel_spmd(nc, in_maps, core_ids=list(range(8)))
    out = np.zeros((4, NBLK, 128, D), np.float32)
    for c in range(8):
        b, half = c // 2, c % 2
        own = tabs[half][1]
        out[b, own] = np.asarray(res.results[c]["out"]).reshape(NOWN, 128, D)
    return out.reshape(4, SEQ, D)
```

```python
import math
import os
from contextlib import ExitStack

import numpy as np
import concourse.bass as bass
import concourse.mybir as mybir
from concourse.bass_utils import run_bass_kernel_spmd

F32 = mybir.dt.float32
BF16 = mybir.dt.bfloat16
AF = mybir.ActivationFunctionType
ALU = mybir.AluOpType
AX = mybir.AxisListType

D = 1024
SEQ = 4096
NBLK = 32
NOWN = 16
TOWN = 2048
NE = 32
NEG = -30000.0
EPS = 1e-6
ALPHA = 1.702
LAMBDA_INIT = 0.8 - 0.6 * math.exp(-0.3 * 0)


class Sched:
    ENGS = ("pe", "act", "dve", "pool", "sp")

    def __init__(self, nc):
        self.nc = nc
        self.prog = {e: [] for e in self.ENGS}
        self.cnt = {}
        self.seen = {e: {} for e in self.ENGS}
        self.lastw = {}
        self.readers = {}
        self.sem_names = set()

    def _deps(self, reads, writes):
        deps = []
        for k in reads:
            t = self.lastw.get(k)
            if t is not None:
                deps.append(t)
        for k in writes:
            t = self.lastw.get(k)
            if t is not None:
                deps.append(t)
            deps.extend(self.readers.get(k, ()))
        return deps

    def _commit(self, ticket, reads, writes):
        for k in reads:
            self.readers.setdefault(k, []).append(ticket)
        for k in writes:
            self.lastw[k] = ticket
            self.readers[k] = []

    def _filter(self, eng, deps):
        best = {}
        for (s, v) in deps:
            if s == "E:pe" and eng == "pe":
                continue
            if v > best.get(s, 0):
                best[s] = v
        out = []
        seen = self.seen[eng]
        for s, v in best.items():
            if seen.get(s, 0) >= v:
                continue
            seen[s] = v
            out.append((s, v))
        return out

    def op(self, eng, fn, reads=(), writes=()):
        waits = self._filter(eng, self._deps(reads, writes))
        s = "E:" + eng
        self.sem_names.add(s)
        self.cnt[s] = self.cnt.get(s, 0) + 1
        ticket = (s, self.cnt[s])
        self.prog[eng].append((waits, fn, (s, 1)))
        self._commit(ticket, reads, writes)
        return ticket

    def dma(self, q, sem, fn, reads=(), writes=()):
        waits = self._filter(q, self._deps(reads, writes))
        s = "D:" + sem
        self.sem_names.add(s)
        self.cnt[s] = self.cnt.get(s, 0) + 16
        ticket = (s, self.cnt[s])
        self.prog[q].append((waits, fn, (s, 16)))
        self._commit(ticket, reads, writes)
        return ticket

    def fence(self):
        allt = list(self.cnt.items())
        for eng in self.ENGS:
            self.prog[eng].append((self._filter(eng, allt), None, None))

    def wait_all(self, eng, keys):
        deps = [self.lastw[k] for k in keys if k in self.lastw]
        self.prog[eng].append((self._filter(eng, deps), None, None))

    def emit(self):
        nc = self.nc
        with ExitStack() as st:
            sems = {}
            for s in sorted(self.sem_names):
                sems[s] = st.enter_context(nc.semaphore(s.replace(":", "_")))
            block = st.enter_context(nc.Block())

            def run(name):
                def body(eng):
                    for waits, fn, inc in self.prog[name]:
                        for (s, v) in waits:
                            eng.wait_ge(sems[s], v)
                        if fn is not None:
                            fn(eng).then_inc(sems[inc[0]], inc[1])
                return body

            block.tensor(run("pe"))
            block.scalar(run("act"))
            block.vector(run("dve"))
            block.gpsimd(run("pool"))
            block.sync(run("sp"))


def own_blocks(half):
    r = (0, 3) if half == 0 else (1, 2)
    own = [j for j in range(NBLK) if j % 4 in r]
    oth = [j for j in range(NBLK) if j % 4 not in r]
    return own, oth


def t5_bucket_np(rel):
    n = np.maximum(rel, 0)
    nf = np.maximum(n, 1).astype(np.float32)
    large = 16 + (np.log(nf / np.float32(16)) / np.float32(math.log(128 / 16)) * np.float32(16)).astype(np.int32)
    large = np.minimum(large, 31)
    return np.where(n < 16, n, large)


def const_tables(half):
    own, oth = own_blocks(half)
    perm = own + oth
    pos = np.concatenate([np.arange(j * 128, (j + 1) * 128) for j in perm]).astype(np.float32)
    inv_freq = (1.0 / (10000.0 ** np.linspace(0.0, 1.0, 32, dtype=np.float32))).astype(np.float32)
    ang = pos[:, None] * np.repeat(inv_freq, 2)[None, :]
    sin = np.sin(ang).astype(np.float32)
    cos = np.cos(ang).astype(np.float32)
    sign = np.where(np.arange(64) % 2 == 0, -1.0, 1.0).astype(np.float32)
    cosT = np.tile(cos.T, (2, 1))
    sinT = np.tile((sin * sign[None, :]).T, (2, 1))
    tab = {}
    tab["rotq"] = np.ascontiguousarray(np.stack([cosT[:, :TOWN], sinT[:, :TOWN]], 1)).astype(np.float32)
    tab["rotk"] = np.ascontiguousarray(np.stack([cosT * 0.125, sinT * 0.125], 1)).astype(np.float32)
    H = 4
    log_g = np.log(1.0 - 2.0 ** (-5.0 - np.arange(H, dtype=np.float64)))
    i = np.arange(128, dtype=np.float64)
    zeta = np.exp((127.0 - i)[:, None] * log_g[None, :])
    xi = np.exp((i + 1.0)[:, None] * log_g[None, :])
    mprime = np.zeros((128, H, 128), np.float64)
    jj = i[:, None]
    ii = i[None, :]
    for h in range(H):
        mprime[:, h, :] = np.where(jj <= ii, np.exp(-(jj + 1.0) * log_g[h]), 0.0)
    tab["zx"] = np.concatenate([zeta, xi], 1).astype(np.float32)
    tab["mprime"] = mprime.astype(np.float32)
    g = np.exp(128.0 * log_g)
    coef = np.zeros((128, 2, 9), np.float64)
    for jp in range(2):
        for p in range(128):
            gh = g[2 * jp + p // 64]
            if half == 0:
                c = [1, 0, gh ** 3, gh ** 2, gh, 1, gh, 1, 0]
            else:
                c = [gh, 1, gh ** 2, 1, gh, 0, gh ** 2, gh, 1]
            coef[p, jp, :] = c
    tab["coef"] = coef.astype(np.float32)
    tab["ident"] = np.eye(128, dtype=np.float32)
    k = 1
    qpos = np.concatenate([np.arange(own[2 * k] * 128, own[2 * k] * 128 + 128),
                           np.arange(own[2 * k + 1] * 128, own[2 * k + 1] * 128 + 128)])
    slots = [own[2 * k - 1], own[2 * k], own[2 * k + 1], oth[2 * k], oth[2 * k + 1]]
    rels = []
    for s in slots:
        kpos = np.arange(s * 128, s * 128 + 128)
        rels.append(qpos[None, :] - kpos[:, None])
    rel = np.stack(rels, 1)
    tab["bias_rel"] = rel
    tab["bias_bucket"] = t5_bucket_np(rel)
    return tab, own, oth


def check_structure():
    for half in (0, 1):
        tab, own, oth = const_tables(half)
        for k in range(8):
            qpos = np.concatenate([np.arange(own[2 * k] * 128, own[2 * k] * 128 + 128),
                                   np.arange(own[2 * k + 1] * 128, own[2 * k + 1] * 128 + 128)])
            special = {}
            if k > 0:
                special[own[2 * k - 1]] = 0
            special[own[2 * k]] = 1
            special[own[2 * k + 1]] = 2
            special[oth[2 * k]] = 3
            special[oth[2 * k + 1]] = 4
            keys = [own[j] for j in range(2 * k + 2)] + [oth[j] for j in range(2 * k + 2)]
            for gb in range(NBLK):
                kpos = np.arange(gb * 128, gb * 128 + 128)
                rel = qpos[None, :] - kpos[:, None]
                if gb in special:
                    s = special[gb]
                    assert np.array_equal(rel, tab["bias_rel"][:, s, :]), (half, k, gb)
                    if s in (2, 4):
                        assert (rel[:, :128] < 0).all()
                elif gb in keys:
                    assert (rel >= 113).all(), (half, k, gb)
                else:
                    assert (rel < 0).all(), (half, k, gb)
    return True


def prep_core(core, inp, tabs):
    b, half = core // 2, core % 2
    tab, own, oth = tabs[half]
    perm = own + oth
    x = inp["x"][b].reshape(NBLK, 128, D)
    m = {}
    m["xp"] = np.ascontiguousarray(x[perm].reshape(SEQ, D))
    m["cT"] = np.ascontiguousarray(inp["c"][b].reshape(8, 128).T)
    m["w_ada"] = inp["w_ada"][0]
    m["b_ada"] = inp["b_ada"][0].reshape(1, 6 * D)
    m["n12T"] = np.ascontiguousarray(np.concatenate([inp["norm1_w"][0].reshape(8, 128).T,
                                                     inp["norm2_w"][0].reshape(8, 128).T], 1))
    m["normfB"] = np.ascontiguousarray(np.broadcast_to(inp["normf_w"][None, :], (128, D)))
    w_in = inp["w_in"][0]
    m["w_in"] = w_in
    sw = w_in[:, :512].reshape(D, 256, 2)[:, :, ::-1].reshape(D, 512)
    m["w_sw"] = np.ascontiguousarray(sw)
    lam = np.concatenate([inp["lam_q1"][0], inp["lam_k1"][0], inp["lam_q2"][0], inp["lam_k2"][0]])
    m["lamB"] = np.ascontiguousarray(np.broadcast_to(lam[None, :], (128, 256)))
    m["sublnB"] = np.ascontiguousarray(np.broadcast_to(inp["subln_w"][0][None, :], (128, 128)))
    rb = inp["rel_bias"]
    bias = rb[tab["bias_bucket"]]
    bias = np.where(tab["bias_rel"][..., None] >= 0, bias, np.float32(NEG)).astype(np.float32)
    m["biasT"] = np.ascontiguousarray(np.transpose(bias, (0, 3, 1, 2)))
    m["cfar"] = np.ascontiguousarray(np.broadcast_to(rb[31][None, :], (128, 4)))
    m["w_out"] = inp["w_out"][0]
    m["wrT"] = np.ascontiguousarray(inp["w_router"][0].reshape(8, 128, NE).transpose(1, 0, 2))
    m["brB"] = np.ascontiguousarray(np.broadcast_to(inp["b_router"][0][None, :], (128, NE)))
    m["w1"] = inp["w1"][0]
    m["w2"] = inp["w2"][0]
    b1 = inp["b1"][0].reshape(NE, 8, 128, 2)
    m["b1T"] = np.ascontiguousarray(b1.transpose(2, 0, 1, 3))
    m["b2"] = inp["b2"][0]
    for k in ("rotq", "rotk", "zx", "mprime", "coef", "ident"):
        m[k] = tab[k]
    return m


def build_program(debug=False, n_experts=NE, n_diff=4, n_ret=2, stop=None):
    nc = bass.Bass("TRN2", target_bir_lowering=False)
    S = Sched(nc)
    dbg_outs = {}

    def din(name, shape):
        return nc.dram_tensor(name, list(shape), F32, kind="ExternalInput").ap()

    xp = din("xp", [SEQ, D])
    cT_d = din("cT", [128, 8])
    w_ada = din("w_ada", [D, 6 * D])
    b_ada = din("b_ada", [1, 6 * D])
    n12T_d = din("n12T", [128, 16])
    normfB_d = din("normfB", [128, D])
    w_in = din("w_in", [D, 3072])
    w_sw = din("w_sw", [D, 512])
    lamB_d = din("lamB", [128, 256])
    sublnB_d = din("sublnB", [128, 128])
    biasT_d = din("biasT", [128, 4, 5, 256])
    cfar_d = din("cfar", [128, 4])
    w_out = din("w_out", [D, D])
    wrT_d = din("wrT", [128, 8, NE])
    brB_d = din("brB", [128, NE])
    w1 = din("w1", [n_experts, D, 2 * D])
    w2 = din("w2", [n_experts, D, D])
    b1T_d = din("b1T", [128, NE, 8, 2])
    b2_d = din("b2", [NE, D])
    rotq_d = din("rotq", [128, 2, TOWN])
    rotk_d = din("rotk", [128, 2, SEQ])
    zx_d = din("zx", [128, 8])
    mprime_d = din("mprime", [128, 4, 128])
    coef_d = din("coef", [128, 2, 9])
    ident_d = din("ident", [128, 128])
    out_d = nc.dram_tensor("out", [TOWN, D], F32, kind="ExternalOutput").ap()

    def dbg_out(name, shape):
        t = nc.dram_tensor("dbg_" + name, list(shape), F32, kind="ExternalOutput").ap()
        dbg_outs[name] = t
        return t

    with ExitStack() as st:
        def sb(name, shape, dt=F32):
            return st.enter_context(nc.sbuf_tensor("s_" + name, list(shape), dt))

        arena1 = sb("arena1", [128, 8 * SEQ], BF16)
        hT = arena1[:, :].rearrange("p (c t) -> p c t", c=8)
        xres = arena1[:, :].bitcast(F32).rearrange("p (t d) -> p t d", t=NOWN)
        yTf = sb("yTf", [128, 8 * TOWN], BF16)
        yT = yTf[:, :].rearrange("p (c t) -> p c t", c=8)
        actT = yT
        wada_buf = [yTf[:, i * 8192:(i + 1) * 8192].bitcast(F32).rearrange("p (c n) -> p c n", c=8) for i in range(2)]
        arena3 = sb("arena3", [128, 30720], BF16)
        KT = arena3[:, 0:4096]
        QT = arena3[:, 4096:6144]
        Vb = arena3[:, 6144:14592].rearrange("p (t c) -> p t c", t=32)
        Gs = arena3[:, 14592:18688].rearrange("p (t c) -> p t c", t=16)
        biasT = arena3[:, 18688:21248].bitcast(F32).rearrange("p (s q) -> p s q", s=5)
        wsl = [arena3[:, 21248 + i * 1024:21248 + (i + 1) * 1024].rearrange("p (c n) -> p c n", c=8) for i in range(4)]
        wsl.append(arena3[:, 25344:27392].rearrange("p (c n) -> p c n", c=8))
        PT = [arena3[:, 27392 + i * 1024:27392 + (i + 1) * 1024] for i in range(3)]
        xsb = arena3[:, 28928:29952]
        h2T = arena3[:, 0:16384].rearrange("p (c t) -> p c t", c=8)
        w2b = arena3[:, 16384:24576].rearrange("p (c n) -> p c n", c=8)
        w1c = [arena3[:, 24576 + i * 2048:24576 + (i + 1) * 2048].rearrange("p (c n) -> p c n", c=8) for i in range(3)]
        arena4 = sb("arena4", [128, 4096], F32)
        rot = arena4[:, 0:1024].rearrange("p (a t) -> p a t", a=2)
        mprime = arena4[:, 1024:1536].rearrange("p (h i) -> p h i", h=4)
        kvc = arena4[:, 1536:2048].rearrange("p (s e) -> p s e", s=4)
        ybufs = arena4[:, 2048:2560].rearrange("p (s e) -> p s e", s=4)
        t0b = arena4[:, 2560:2816].rearrange("p (s e) -> p s e", s=2)
        Rst = arena4[:, 2816:2944]
        Ro = [arena4[:, 2944:3072], arena4[:, 3072:3200]]
        Rb = [arena4[:, 3200:3264].bitcast(BF16), arena4[:, 3264:3328].bitcast(BF16)]
        SM = [arena4[:, 3328:3392].bitcast(BF16), arena4[:, 3392:3456].bitcast(BF16),
              arena4[:, 3840:3904].bitcast(BF16), arena4[:, 3904:3968].bitcast(BF16)]
        yo = [arena4[:, 3456:3520].bitcast(BF16), arena4[:, 3520:3584].bitcast(BF16)]
        Kz4 = arena4[:, 3584:3840].bitcast(BF16).rearrange("p (s c) -> p s c", s=4)
        b1T = arena4[:, 0:512].rearrange("p (e c g) -> p e c g", e=NE, c=8)
        b1l7s = arena4[:, 512:768].rearrange("p (e c) -> p e c", e=NE)
        b2s = arena4[0:NE, 768:1792]
        gates = arena4[:, 1792:2304].rearrange("p (t e) -> p t e", t=NOWN)
        wrT = arena4[:, 2304:2560].rearrange("p (c e) -> p c e", c=8)
        gT = arena4[0:NE, 2560:2688]
        logit = arena4[:, 2688:2720]
        top8 = arena4[:, 2720:2728]
        rtmp = arena4[:, 2728:2824].rearrange("p (a e) -> p a e", a=3)
        brB = arena4[:, 2824:2856]
        gate12B = sb("gate12B", [128, 2, D])
        xin = [sb(f"xin{i}", [128, D]) for i in range(2)]
        tmpA = sb("tmpA", [128, D])
        tmpB = sb("tmpB", [128, D])
        tmpC = sb("tmpC", [128, D])
        junk = sb("junk", [128, D], BF16)
        identf = sb("identf", [128, 128])
        identb = sb("identb", [128, 128], BF16)
        small = sb("small", [128, 256])
        sublnB = sb("sublnB", [128, 128])
        cfar = sb("cfar", [128, 4])
        zx = sb("zx", [128, 8])
        coef = sb("coef", [128, 2, 9])
        psall = st.enter_context(nc.psum_tensor("psall", [128, 4096], F32))
        ps = [psall[:, i * 512:(i + 1) * 512] for i in range(8)]

        def ps_pair(b0):
            return psall[:, b0 * 512:(b0 + 2) * 512].rearrange("p (m q) -> p m q", m=2)[:, :, 0:256]

        _o = [0]

        def sc(n=1):
            v = small[:, _o[0]:_o[0] + n]
            _o[0] += n
            return v

        condT = sc(8)
        cTs = sc(8)
        n12T = sc(16)
        modT = sc(32)
        A1T = sc(8)
        A2T = sc(8)
        ssq = sc(32)
        sdv = sc(32)
        rstd = sc(32)
        epsc = sc(1)
        lamv = sc(4)
        neglam = sc(1)
        est = sc(16)
        onesr = sc(1)

        def PK(b):
            return ("ps", b)

        def dma(q, sem, out, in_, reads=(), writes=()):
            return S.dma(q, sem, lambda e, o_=out, i_=in_: e.dma_start(out=o_, in_=i_), reads=reads, writes=writes)

        def ld(name, dst, src):
            dma("sp", "c_" + name, dst, src, writes=[name])

        ld("identf", identf[:], ident_d[:, :])
        ld("cTs", cTs, cT_d[:, :])
        ld("n12T", n12T, n12T_d[:, :])
        ld("tmpA", tmpA[:, 0:256], lamB_d[:, :])
        ld("sublnB", sublnB[:], sublnB_d[:, :])
        ld("cfar", cfar[:], cfar_d[:, :])
        ld("zx", zx[:], zx_d[:, :])
        ld("mprime", mprime, mprime_d[:, :, :])
        ld("coef", coef[:], coef_d[:, :, :])
        S.op("dve", lambda e: e.tensor_copy(out=identb[:], in_=identf[:]), reads=["identf"], writes=["identb"])
        S.op("dve", lambda e: e.memset(epsc, EPS), writes=["epsc"])
        S.op("dve", lambda e: e.memset(ssq, 0.0), writes=[("ssq", t) for t in range(32)])
        S.op("dve", lambda e: e.tensor_scalar(out=sublnB[:], in0=sublnB[:], scalar1=1.0 - LAMBDA_INIT, scalar2=None, op0=ALU.mult),
             reads=["sublnB"], writes=["sublnB"])
        S.op("dve", lambda e: e.tensor_tensor(out=tmpB[:, 0:64], in0=tmpA[:, 0:64], in1=tmpA[:, 64:128], op=ALU.mult),
             reads=["tmpA"], writes=["tmpB"])
        S.op("dve", lambda e: e.tensor_tensor(out=tmpB[:, 64:128], in0=tmpA[:, 128:192], in1=tmpA[:, 192:256], op=ALU.mult),
             reads=["tmpA"], writes=["tmpB"])
        S.op("dve", lambda e: e.tensor_reduce(out=lamv[:, 0:2], in_=tmpB[:, 0:128].rearrange("p (a b) -> p a b", a=2),
                                              axis=AX.X, op=ALU.add), reads=["tmpB"], writes=["lamv"])
        S.op("act", lambda e: e.activation(out=lamv[:, 2:4], in_=lamv[:, 0:2], func=AF.Exp), reads=["lamv"], writes=["lamv"])
        S.op("dve", lambda e: e.tensor_tensor(out=neglam, in0=lamv[:, 3:4], in1=lamv[:, 2:3], op=ALU.subtract),
             reads=["lamv"], writes=["neglam"])
        S.op("dve", lambda e: e.tensor_scalar(out=neglam, in0=neglam, scalar1=-LAMBDA_INIT, scalar2=None, op0=ALU.add),
             reads=["neglam"], writes=["neglam"])

        S.op("act", lambda e: e.activation(out=condT, in_=cTs, func=AF.Silu), reads=["cTs"], writes=["condT"])
        S.op("dve", lambda e: e.memset(tmpB[0:1, 256:384], 1.0), writes=["tmpB"])
        ones_row = tmpB[0:1, 256:384]
        modrow = tmpC[0:1, 0:512]
        badar = tmpC[0:1, 512:1024]
        for fb in range(12):
            mi, half_ = fb // 2, fb % 2
            buf = wada_buf[fb % 2]
            for hh in range(2):
                dma("sp", f"wada{fb % 2}", buf[:, hh * 4:(hh + 1) * 4, :],
                    w_ada[hh * 512:(hh + 1) * 512, fb * 512:(fb + 1) * 512].rearrange("(c p) n -> p c n", p=128),
                    writes=[("wada", fb % 2)])
            dma("sp", "badar", badar, b_ada[0:1, fb * 512:(fb + 1) * 512], writes=["badar"])
            bank = ps[fb % 2]

            def mm(e, buf=buf, bank=bank):
                ins = None
                for kc in range(8):
                    ins = e.matmul(bank[0:1, :], lhsT=condT[:, kc:kc + 1], rhs=buf[:, kc, :], start=(kc == 0), stop=(kc == 7))
                return ins
            S.op("pe", mm, reads=["condT", ("wada", fb % 2)], writes=[PK(fb % 2)])
            S.op("dve", lambda e, bank=bank: e.tensor_tensor(out=modrow, in0=bank[0:1, :], in1=badar, op=ALU.add),
                 reads=[PK(fb % 2), "badar"], writes=["modrow"])
            if mi in (2, 5):
                gi = 0 if mi == 2 else 1
                S.op("pe", lambda e: e.matmul(ps[2][:, :], lhsT=ones_row, rhs=modrow, start=True, stop=True),
                     reads=["tmpB", "modrow"], writes=[PK(2)])
                S.op("act", lambda e, gi=gi, half_=half_: e.copy(out=gate12B[:, gi, half_ * 512:(half_ + 1) * 512], in_=ps[2][:, :]),
                     reads=[PK(2)], writes=["gate12B"])
            else:
                j = (0, 1, None, 2, 3)[mi]

                def mmT(e, j=j, half_=half_):
                    ins = None
                    for c4 in range(4):
                        col = j * 8 + half_ * 4 + c4
                        ins = e.matmul(ps[3][:, col:col + 1], lhsT=modrow[0:1, c4 * 128:(c4 + 1) * 128], rhs=ones_row[0:1, 0:1], start=True, stop=True)
                    return ins
                S.op("pe", mmT, reads=["modrow", "tmpB"], writes=[PK(3)])
                S.op("dve", lambda e, j=j, half_=half_: e.tensor_copy(out=modT[:, j * 8 + half_ * 4:j * 8 + half_ * 4 + 4],
                                                                      in_=ps[3][:, j * 8 + half_ * 4:j * 8 + half_ * 4 + 4]),
                     reads=[PK(3)], writes=["modT"])
        S.op("dve", lambda e: e.scalar_tensor_tensor(out=A1T, in0=modT[:, 8:16], scalar=1.0, in1=n12T[:, 0:8], op0=ALU.add, op1=ALU.mult),
             reads=["modT", "n12T"], writes=["A1T"])
        S.op("dve", lambda e: e.scalar_tensor_tensor(out=A2T, in0=modT[:, 24:32], scalar=1.0, in1=n12T[:, 8:16], op0=ALU.add, op1=ALU.mult),
             reads=["modT", "n12T"], writes=["A2T"])
        shift1T = modT[:, 0:8]
        shift2T = modT[:, 16:24]

        def bc(v, n):
            return v.unsqueeze(2).to_broadcast([128, n, 128])

        def hkey(c, tg):
            return ("hT", c, tg)

        if stop == 'p0':
            S.fence(); S.emit(); return nc, dbg_outs
        for t in range(NBLK):
            xb_ = xin[t % 2]
            dma("sp", f"xin{t % 2}", xb_[:], xp[t * 128:(t + 1) * 128, :], writes=[("xin", t % 2)])
            S.op("act", lambda e, xb_=xb_, t=t: e.activation(out=junk[:], in_=xb_[:], func=AF.Square, accum_out=ssq[:, t:t + 1]),
                 reads=[("xin", t % 2), ("ssq", t)], writes=["junk", ("ssq", t)])
            S.op("act", lambda e, t=t: e.activation(out=sdv[:, t:t + 1], in_=ssq[:, t:t + 1], func=AF.Sqrt, scale=1.0 / D, bias=epsc),
                 reads=[("ssq", t), "epsc"], writes=[("sdv", t)])
            S.op("dve", lambda e, t=t: e.reciprocal(out=rstd[:, t:t + 1], in_=sdv[:, t:t + 1]), reads=[("sdv", t)], writes=[("rstd", t)])
            S.op("act", lambda e, xb_=xb_, t=t: e.activation(out=xsb, in_=xb_[:], func=AF.Copy, scale=rstd[:, t:t + 1]),
                 reads=[("xin", t % 2), ("rstd", t)], writes=["xsb"])
            bi = 4 + t % 2
            bv = ps[bi][:, :].bitcast(BF16).rearrange("p (c t) -> p c t", c=8)

            def tr(e, bv=bv):
                ins = None
                for c in range(8):
                    ins = e.transpose(out=bv[:, c, :], in_=xsb[:, c * 128:(c + 1) * 128], identity=identb[:])
                return ins
            S.op("pe", tr, reads=["xsb", "identb"], writes=[PK(bi)])
            tv = tmpA[:, :].rearrange("p (c t) -> p c t", c=8)
            S.op("dve", lambda e, bv=bv, tv=tv: e.tensor_tensor(out=tv, in0=bv, in1=bc(A1T, 8), op=ALU.mult),
                 reads=[PK(bi), "A1T"], writes=["tmpA"])
            S.op("dve", lambda e, tv=tv, t=t: e.tensor_tensor(out=hT[:, :, t * 128:(t + 1) * 128], in0=tv, in1=bc(shift1T, 8), op=ALU.add),
                 reads=["tmpA", "modT"], writes=[hkey(c, t // 4) for c in range(8)])

        if debug:
            d = dbg_out("hT", [128, 8, SEQ])
            for c in range(8):
                for q4 in range(4):
                    S.op("dve", lambda e, c=c, q4=q4: e.tensor_copy(out=tmpA[:], in_=hT[:, c, q4 * 1024:(q4 + 1) * 1024]),
                         reads=[hkey(c, tg) for tg in range(8)], writes=["tmpA"])
                    dma("sp", "dbg", d[:, c, q4 * 1024:(q4 + 1) * 1024], tmpA[:], reads=["tmpA"], writes=["dbgout"])

        if stop == 'p1':
            S.fence(); S.emit(); return nc, dbg_outs
        def load_w(slot, src_ap, ncols):
            dma("pool", f"wsl{slot}", wsl[slot][:, :, 0:ncols], src_ap.rearrange("(c p) n -> p c n", p=128), writes=[("wsl", slot)])

        def proj_fm(slot, tg, bank_i):
            bank = ps[bank_i]

            def mm(e):
                ins = None
                for kc in range(8):
                    ins = e.matmul(bank[:, :], lhsT=wsl[slot][:, kc, 0:128], rhs=hT[:, kc, tg * 512:(tg + 1) * 512],
                                   start=(kc == 0), stop=(kc == 7))
                return ins
            S.op("pe", mm, reads=[("wsl", slot)] + [hkey(c, tg) for c in range(8)], writes=[PK(bank_i)])

        def proj_tm(slot, ncols, t0_, ntiles, bank_i):
            bank = ps[bank_i]

            def mm(e):
                ins = None
                for j in range(ntiles):
                    t = t0_ + j
                    for kc in range(8):
                        ins = e.matmul(bank[:, j * ncols:(j + 1) * ncols], lhsT=hT[:, kc, t * 128:(t + 1) * 128], rhs=wsl[slot][:, kc, 0:ncols],
                                       start=(kc == 0), stop=(kc == 7))
                return ins
            S.op("pe", mm, reads=[("wsl", slot)] + [hkey(c, tg) for c in range(8) for tg in sorted({(t0_ + j) // 4 for j in range(ntiles)})],
                 writes=[PK(bank_i)])

        def rotary_proj(slot_a, slot_b, tg, table_d, dst, dst_key):
            dma("sp", "rot", rot[:, :, :], table_d[:, :, tg * 512:(tg + 1) * 512], writes=["rot"])
            proj_fm(slot_a, tg, 0)
            proj_fm(slot_b, tg, 1)
            S.op("dve", lambda e: e.tensor_tensor(out=tmpA[:, 0:512], in0=ps[0][:, :], in1=rot[:, 0, :], op=ALU.mult),
                 reads=[PK(0), "rot"], writes=["tmpA"])
            S.op("dve", lambda e: e.tensor_tensor(out=tmpB[:, 0:512], in0=ps[1][:, :], in1=rot[:, 1, :], op=ALU.mult),
                 reads=[PK(1), "rot"], writes=["tmpB"])
            S.op("dve", lambda e: e.tensor_tensor(out=dst[:, tg * 512:(tg + 1) * 512], in0=tmpA[:, 0:512], in1=tmpB[:, 0:512], op=ALU.add),
                 reads=["tmpA", "tmpB"], writes=[(dst_key, tg)])

        ep_cnt = [0]

        def rms_epilogue(y_ap, mult_ap, chunk, tok_lo, ykey, mkey):
            if len(pending) >= 2:
                flush_pending()
            i = ep_cnt[0] % 2
            ep_cnt[0] += 1
            e0 = est[:, 4 * i:4 * i + 1]
            e1 = est[:, 4 * i + 1:4 * i + 2]
            e2 = est[:, 4 * i + 2:4 * i + 3]
            ek = ("est", i)
            S.op("dve", lambda e: e.memset(e0, 0.0), writes=[ek])
            S.op("act", lambda e: e.activation(out=junk[:, 0:128], in_=y_ap, func=AF.Square, accum_out=e0),
                 reads=[ykey, ek], writes=["junk", ek])
            S.op("act", lambda e: e.activation(out=e1, in_=e0, func=AF.Sqrt, scale=1.0 / 128, bias=epsc), reads=[ek, "epsc"], writes=[ek])
            S.op("dve", lambda e: e.reciprocal(out=e2, in_=e1), reads=[ek], writes=[ek])
            S.op("dve", lambda e: e.scalar_tensor_tensor(out=yo[i], in0=y_ap, scalar=e2, in1=mult_ap, op0=ALU.mult, op1=ALU.mult),
                 reads=[ykey, ek, mkey], writes=[("yo", i)])
            bv = ps[i][:, :].bitcast(BF16)

            def part_b():
                S.op("pe", lambda e: e.transpose(out=bv[:, 0:128], in_=yo[i], identity=identb[:]),
                     reads=[("yo", i), "identb"], writes=[PK(i)])
                S.op("act", lambda e: e.copy(out=yT[:, chunk, tok_lo:tok_lo + 128], in_=bv[:, 0:128]),
                     reads=[PK(i)], writes=[("yT", chunk, tok_lo // 128)])
            pending.append(part_b)

        pending = []

        def flush_pending():
            while pending:
                pending.pop(0)()

        def ret_group(jp, k):
            cf = [coef[:, jp, i:i + 1] for i in range(9)]
            blocks = [2 * k, 2 * k + 1, 16 + 2 * k, 17 + 2 * k]
            bv = ps[4][:, :].bitcast(BF16)

            def trk(e):
                ins = None
                for si, n in enumerate(blocks):
                    ins = e.transpose(out=bv[:, si * 128:(si + 1) * 128], in_=KT[:, n * 128:(n + 1) * 128], identity=identb[:])
                return ins
            S.op("pe", trk, reads=[("KT", n // 4) for n in blocks] + ["identb"], writes=[PK(4)])
            for hh in range(2):
                h = 2 * jp + hh
                S.op("act", lambda e, hh=hh, h=h: e.activation(out=Kz4[:, :, hh * 64:(hh + 1) * 64],
                                                               in_=bv[:, 0:512].rearrange("p (s c) -> p s c", s=4)[:, :, hh * 64:(hh + 1) * 64],
                                                               func=AF.Copy, scale=zx[:, h:h + 1]),
                     reads=[PK(4), "zx"], writes=[("Kz4", hh)])
            for si, n in enumerate(blocks):
                bi = 5 + si // 2
                off = (si % 2) * 256

                def mmkv(e, n=n, bi=bi, off=off, si=si):
                    e.matmul(ps[bi][:, off:off + 128], lhsT=Kz4[:, si, :], rhs=Vb[:, n, 0:128], start=True, stop=True)
                    return e.matmul(ps[bi][:, off + 128:off + 256], lhsT=Kz4[:, si, :], rhs=Vb[:, n, 128:256], start=True, stop=True)
                S.op("pe", mmkv, reads=[("Kz4", 0), ("Kz4", 1), ("V", n)], writes=[PK(bi)])
                S.op("act", lambda e, si=si, bi=bi, off=off: e.copy(out=kvc[0:64, si, :], in_=ps[bi][0:64, off:off + 128]),
                     reads=[PK(bi)], writes=[("kvc", si)])
                S.op("act", lambda e, si=si, bi=bi, off=off: e.copy(out=kvc[64:128, si, :], in_=ps[bi][64:128, off + 128:off + 256]),
                     reads=[PK(bi)], writes=[("kvc", si)])
            T = [tmpC[:, i * 128:(i + 1) * 128] for i in range(6)]
            S.op("dve", lambda e: e.tensor_scalar(out=T[0], in0=kvc[:, 2, :], scalar1=cf[1], scalar2=None, op0=ALU.mult),
                 reads=[("kvc", 2), "coef"], writes=["tmpC"])
            S.op("dve", lambda e: e.scalar_tensor_tensor(out=Ro[0], in0=Rst, scalar=cf[0], in1=T[0], op0=ALU.mult, op1=ALU.add),
                 reads=["Rst", "tmpC", "coef"], writes=[("Ro", 0)])
            S.op("dve", lambda e: e.tensor_scalar(out=T[1], in0=kvc[:, 0, :], scalar1=cf[3], scalar2=None, op0=ALU.mult),
                 reads=[("kvc", 0), "coef"], writes=["tmpC"])
            S.op("dve", lambda e: e.scalar_tensor_tensor(out=T[2], in0=kvc[:, 2, :], scalar=cf[4], in1=T[1], op0=ALU.mult, op1=ALU.add),
                 reads=[("kvc", 2), "tmpC", "coef"], writes=["tmpC"])
            S.op("dve", lambda e: e.scalar_tensor_tensor(out=T[3], in0=kvc[:, 3, :], scalar=cf[5], in1=T[2], op0=ALU.mult, op1=ALU.add),
                 reads=[("kvc", 3), "tmpC", "coef"], writes=["tmpC"])
            S.op("dve", lambda e: e.scalar_tensor_tensor(out=Ro[1], in0=Rst, scalar=cf[2], in1=T[3], op0=ALU.mult, op1=ALU.add),
                 reads=["Rst", "tmpC", "coef"], writes=[("Ro", 1)])
            S.op("dve", lambda e: e.tensor_scalar(out=T[4], in0=kvc[:, 1, :], scalar1=cf[7], scalar2=None, op0=ALU.mult),
                 reads=[("kvc", 1), "coef"], writes=["tmpC"])
            S.op("dve", lambda e: e.scalar_tensor_tensor(out=T[5], in0=kvc[:, 3, :], scalar=cf[8], in1=T[4], op0=ALU.mult, op1=ALU.add),
                 reads=[("kvc", 3), "tmpC", "coef"], writes=["tmpC"])
            S.op("dve", lambda e: e.scalar_tensor_tensor(out=Rst, in0=Ro[1], scalar=cf[6], in1=T[5], op0=ALU.mult, op1=ALU.add),
                 reads=[("Ro", 1), "tmpC", "coef"], writes=["Rst"])
            for qi in range(2):
                S.op("act", lambda e, qi=qi: e.copy(out=Rb[qi], in_=Ro[qi]), reads=[("Ro", qi)], writes=[("Rb", qi)])
            combos = [(qi, hh) for qi in range(2) for hh in range(2)]
            for qi, hh in combos:
                i = 2 * k + qi
                pr = slice(hh * 64, (hh + 1) * 64)
                S.op("pe", lambda e, hh=hh, pr=pr, i=i, qi=qi: e.matmul(ps[2 + hh][:, qi * 256:qi * 256 + 128], lhsT=KT[pr, i * 128:(i + 1) * 128],
                                                                    rhs=QT[pr, i * 128:(i + 1) * 128], start=True, stop=True),
                     reads=[("KT", i // 4), ("QT", i // 4)], writes=[PK(2 + hh)])
            flush_pending()
            for qi, hh in combos:
                h = 2 * jp + hh
                ci = 2 * qi + hh
                S.op("dve", lambda e, hh=hh, h=h, qi=qi, ci=ci: e.tensor_tensor(out=SM[ci], in0=ps[2 + hh][:, qi * 256:qi * 256 + 128], in1=mprime[:, h, :], op=ALU.mult),
                     reads=[PK(2 + hh), "mprime"], writes=[("SM", ci)])
            for qi, hh in combos:
                i = 2 * k + qi
                ci = 2 * qi + hh
                pr = slice(hh * 64, (hh + 1) * 64)

                def mmacc(e, hh=hh, pr=pr, i=i, qi=qi, ci=ci):
                    o = qi * 256 + 128
                    e.matmul(ps[2 + hh][:, o:o + 128], lhsT=QT[pr, i * 128:(i + 1) * 128], rhs=Rb[qi][pr, :], start=True, stop=False)
                    return e.matmul(ps[2 + hh][:, o:o + 128], lhsT=SM[ci], rhs=Vb[:, i, hh * 128:(hh + 1) * 128], start=False, stop=True)
                S.op("pe", mmacc, reads=[("QT", i // 4), ("Rb", qi), ("SM", ci), ("V", i)], writes=[PK(2 + hh)])
            for qi, hh in combos:
                i = 2 * k + qi
                h = 2 * jp + hh
                ci = 2 * qi + hh
                yb = ybufs[:, ci, :]
                yk = ("ybuf", ci)
                S.op("dve", lambda e, hh=hh, yb=yb, h=h, qi=qi: e.tensor_scalar(out=yb, in0=ps[2 + hh][:, qi * 256 + 128:qi * 256 + 256], scalar1=zx[:, 4 + h:5 + h],
                                                                               scalar2=None, op0=ALU.mult),
                     reads=[PK(2 + hh), "zx"], writes=[yk])
                rms_epilogue(yb, Gs[:, i, hh * 128:(hh + 1) * 128], h, i * 128, yk, "Gs")

        for jp in range(n_ret):
            load_w(0, w_in[:, jp * 128:(jp + 1) * 128], 128)
            load_w(1, w_sw[:, jp * 128:(jp + 1) * 128], 128)
            load_w(2, w_in[:, 256 + jp * 128:256 + (jp + 1) * 128], 128)
            load_w(3, w_sw[:, 256 + jp * 128:256 + (jp + 1) * 128], 128)
            load_w(4, w_in[:, 512 + jp * 256:512 + (jp + 1) * 256], 256)
            for tg in range(8):
                rotary_proj(2, 3, tg, rotk_d, KT, "KT")
            for tg in range(4):
                rotary_proj(0, 1, tg, rotq_d, QT, "QT")
            for t2 in range(NBLK // 2):
                bi = 2 + t2 % 2
                proj_tm(4, 256, 2 * t2, 2, bi)
                S.op("act", lambda e, t2=t2, bi=bi: e.copy(out=Vb[:, 2 * t2:2 * t2 + 2, 0:256], in_=ps[bi][:, :].rearrange("p (j c) -> p j c", j=2)),
                     reads=[PK(bi)], writes=[("V", 2 * t2), ("V", 2 * t2 + 1)])
            load_w(4, w_in[:, 1024 + jp * 256:1024 + (jp + 1) * 256], 256)
            for t2 in range(NOWN // 2):
                bi = 2 + t2 % 2
                proj_tm(4, 256, 2 * t2, 2, bi)
                S.op("act", lambda e, t2=t2, bi=bi: e.activation(out=Gs[:, 2 * t2:2 * t2 + 2, :], in_=ps[bi][:, :].rearrange("p (j c) -> p j c", j=2), func=AF.Silu),
                     reads=[PK(bi)], writes=["Gs"])
            S.op("dve", lambda e: e.memset(Rst, 0.0), writes=["Rst"])
            for k in range(8):
                ret_group(jp, k)
            flush_pending()

        def dump_yT():
            d = dbg_out("yT", [128, 8, TOWN])
            for c in range(8):
                for q2 in range(2):
                    S.op("dve", lambda e, c=c, q2=q2: e.tensor_copy(out=tmpA[:], in_=yT[:, c, q2 * 1024:(q2 + 1) * 1024]),
                         reads=[("yT", c, t) for t in range(NOWN)], writes=["tmpA"])
                    dma("sp", "dbg", d[:, c, q2 * 1024:(q2 + 1) * 1024], tmpA[:], reads=["tmpA"], writes=["dbgout"])

        if stop == 'ret':
            if debug:
                dump_yT()
            S.fence(); S.emit(); return nc, dbg_outs
        def diff_group(h, k):
            far = [j for j in range(2 * k - 1)] + [16 + j for j in range(2 * k)]
            spec = ([(2 * k - 1, 0)] if k > 0 else []) + [(2 * k, 1), (16 + 2 * k, 3), (2 * k + 1, 2), (17 + 2 * k, 4)]
            steps = [(kb, None) for kb in far] + spec
            nst = len(steps)
            assert nst % 2 == 0
            npair = nst // 2
            last_q0 = max(i for i, (kb, s) in enumerate(steps) if s not in (2, 4))

            def stage_s(pi):
                b0 = 2 + 2 * (pi % 2)
                reg = psall[:, b0 * 512:(b0 + 2) * 512].rearrange("p (m j q) -> p m j q", m=2, j=2)

                def mm(e):
                    ins = None
                    for j in range(2):
                        kb = steps[2 * pi + j][0]
                        for m in range(2):
                            pr = slice(m * 64, (m + 1) * 64)
                            ins = e.matmul(ps[b0 + m][:, j * 256:(j + 1) * 256], lhsT=KT[pr, kb * 128:(kb + 1) * 128],
                                           rhs=QT[pr, k * 256:(k + 1) * 256], start=True, stop=True)
                    return ins
                S.op("pe", mm, reads=[("KT", steps[2 * pi + j][0] // 4) for j in range(2)] + [("QT", k // 2)], writes=[PK(b0), PK(b0 + 1)])
                pt = PT[pi % 3]
                ptk = ("PT", pi % 3)
                if steps[2 * pi][1] is None and steps[2 * pi + 1][1] is None:
                    S.op("act", lambda e: e.activation(out=pt.rearrange("p (j m q) -> p m j q", j=2, m=2), in_=reg, func=AF.Exp,
                                                       scale=0.125, bias=cfar[:, h:h + 1]),
                         reads=[PK(b0), PK(b0 + 1), "cfar"], writes=[ptk])
                else:
                    for j in range(2):
                        s = steps[2 * pi + j][1]
                        tv = tmpA[:, j * 512:(j + 1) * 512].rearrange("p (m q) -> p m q", m=2)
                        src = reg[:, :, j, :]
                        if s is None:
                            S.op("dve", lambda e, tv=tv, src=src: e.tensor_scalar(out=tv, in0=src, scalar1=0.125, scalar2=cfar[:, h:h + 1],
                                                                                  op0=ALU.mult, op1=ALU.add),
                                 reads=[PK(b0), PK(b0 + 1), "cfar"], writes=["tmpA"])
                        else:
                            S.op("dve", lambda e, tv=tv, src=src, s=s: e.scalar_tensor_tensor(out=tv, in0=src, scalar=0.125,
                                                                                            in1=biasT[:, s, :].unsqueeze(1).to_broadcast([128, 2, 256]),
                                                                                            op0=ALU.mult, op1=ALU.add),
                                 reads=[PK(b0), PK(b0 + 1), "biasT"], writes=["tmpA"])
                    S.op("act", lambda e: e.activation(out=pt, in_=tmpA[:, :], func=AF.Exp), reads=["tmpA"], writes=[ptk])

            def stage_pv(pi):
                pt = PT[pi % 3]

                def mm(e):
                    ins = None
                    for j in range(2):
                        fi = 2 * pi + j
                        kb, s = steps[fi]
                        for m in range(2):
                            for qi in range(2):
                                if qi == 0 and s in (2, 4):
                                    continue
                                lastidx = last_q0 if qi == 0 else nst - 1
                                ins = e.matmul(ps[6 + m][:, qi * 129:(qi + 1) * 129],
                                               lhsT=pt[:, j * 512 + m * 256 + qi * 128:j * 512 + m * 256 + (qi + 1) * 128],
                                               rhs=Vb[:, kb, 0:129], start=(fi == 0 and qi == 0), stop=(fi == lastidx), skip_group_check=True)
                    return ins
                S.op("pe", mm, reads=[("PT", pi % 3)] + [("V", steps[2 * pi + j][0]) for j in range(2)], writes=[PK(6), PK(7)])

            stage_s(0)
            for pi in range(npair):
                if pi + 1 < npair:
                    stage_s(pi + 1)
                if pi == 0:
                    flush_pending()
                stage_pv(pi)
            for qi in range(2):
                i = 2 * k + qi
                ei = est[:, 8 + 3 * qi: 8 + 3 * qi + 3]
                ek = ("est2", qi)
                S.op("dve", lambda e, ei=ei, qi=qi: e.reciprocal(out=ei[:, 0:1], in_=ps[6][:, qi * 129 + 128:qi * 129 + 129]),
                     reads=[PK(6)], writes=[ek])
                S.op("dve", lambda e, ei=ei, qi=qi: e.reciprocal(out=ei[:, 1:2], in_=ps[7][:, qi * 129 + 128:qi * 129 + 129]),
                     reads=[PK(7)], writes=[ek])
                S.op("dve", lambda e, ei=ei: e.tensor_tensor(out=ei[:, 2:3], in0=ei[:, 1:2], in1=neglam, op=ALU.mult),
                     reads=[ek, "neglam"], writes=[ek])
                t0 = t0b[:, qi, :]
                S.op("dve", lambda e, ei=ei, qi=qi, t0=t0: e.tensor_scalar(out=t0, in0=ps[6][:, qi * 129:qi * 129 + 128], scalar1=ei[:, 0:1], scalar2=None, op0=ALU.mult),
                     reads=[PK(6), ek], writes=[("t0", qi)])
                yb = ybufs[:, qi, :]
                yk = ("ybuf", qi)
                S.op("dve", lambda e, ei=ei, qi=qi, t0=t0, yb=yb: e.scalar_tensor_tensor(out=yb, in0=ps[7][:, qi * 129:qi * 129 + 128], scalar=ei[:, 2:3],
                                                                                       in1=t0, op0=ALU.mult, op1=ALU.add),
                     reads=[PK(7), ek, ("t0", qi)], writes=[yk])
                rms_epilogue(yb, sublnB[:], 4 + h, i * 128, yk, "sublnB")

        for h in range(n_diff):
            load_w(0, w_in[:, 1536 + h * 128:1536 + (h + 1) * 128], 128)
            load_w(2, w_in[:, 2048 + h * 128:2048 + (h + 1) * 128], 128)
            load_w(1, w_in[:, 2560 + h * 128:2560 + (h + 1) * 128], 128)
            dma("sp", "biasT", biasT, biasT_d[:, h, :, :], writes=["biasT"])
            for tg in range(8):
                proj_fm(2, tg, tg % 2)
                S.op("act", lambda e, tg=tg: e.copy(out=KT[:, tg * 512:(tg + 1) * 512], in_=ps[tg % 2][:, :]),
                     reads=[PK(tg % 2)], writes=[("KT", tg)])
            for tg in range(4):
                proj_fm(0, tg, tg % 2)
                S.op("act", lambda e, tg=tg: e.copy(out=QT[:, tg * 512:(tg + 1) * 512], in_=ps[tg % 2][:, :]),
                     reads=[PK(tg % 2)], writes=[("QT", tg)])
            for t4 in range(NBLK // 4):
                bi = 2 + t4 % 2
                proj_tm(1, 128, 4 * t4, 4, bi)
                S.op("act", lambda e, t4=t4, bi=bi: e.copy(out=Vb[:, 4 * t4:4 * t4 + 4, 0:128], in_=ps[bi][:, :].rearrange("p (j c) -> p j c", j=4)),
                     reads=[PK(bi)], writes=[("V", 4 * t4 + j) for j in range(4)])
            S.op("dve", lambda e: e.memset(Vb[:, :, 128:129], 1.0), writes=[("V", t) for t in range(NBLK)])
            for k in range(int(os.environ.get('DIFF_K', '8'))):
                diff_group(h, k)
            flush_pending()

        if debug:
            dump_yT()

        if stop == 'diff':
            S.fence(); S.emit(); return nc, dbg_outs
        S.fence()
        ld("wrT", wrT, wrT_d[:, :, :])
        ld("brB", brB, brB_d[:, :])
        ld("b1T", b1T, b1T_d[:, :, :, :])
        ld("b2s", b2s, b2_d[:, :])
        for hh in range(2):
            dma("pool", "w2b", w2b[:, hh * 4:(hh + 1) * 4, :], w_out[hh * 512:(hh + 1) * 512, :].rearrange("(c p) n -> p c n", p=128),
                writes=["w2b"])
        for t in range(NOWN):
            xb_ = xin[t % 2]
            dma("sp", f"xin{t % 2}", xb_[:], xp[t * 128:(t + 1) * 128, :], writes=[("xin", t % 2)])
            for nh in range(2):
                bank = ps[nh]

                def mm(e, bank=bank, nh=nh, t=t):
                    ins = None
                    for kc in range(8):
                        ins = e.matmul(bank[:, :], lhsT=yT[:, kc, t * 128:(t + 1) * 128], rhs=w2b[:, kc, nh * 512:(nh + 1) * 512],
                                       start=(kc == 0), stop=(kc == 7))
                    return ins
                S.op("pe", mm, reads=[("yT", c, t) for c in range(8)] + ["w2b"], writes=[PK(nh)])
                S.op("dve", lambda e, bank=bank, nh=nh: e.tensor_tensor(out=tmpA[:, nh * 512:(nh + 1) * 512], in0=bank[:, :],
                                                                       in1=gate12B[:, 0, nh * 512:(nh + 1) * 512], op=ALU.mult),
                     reads=[PK(nh), "gate12B"], writes=["tmpA"])
                S.op("dve", lambda e, xb_=xb_, nh=nh, t=t: e.tensor_tensor(out=xres[:, t, nh * 512:(nh + 1) * 512], in0=xb_[:, nh * 512:(nh + 1) * 512],
                                                                          in1=tmpA[:, nh * 512:(nh + 1) * 512], op=ALU.add),
                     reads=[("xin", t % 2), "tmpA"], writes=[("xres", t)])

        if debug:
            d = dbg_out("x1", [TOWN, D])
            for t in range(NOWN):
                dma("sp", "dbg", d[t * 128:(t + 1) * 128, :], xres[:, t, :], reads=[("xres", t)], writes=["dbgout"])

        def norm_stats(t, col):
            S.op("dve", lambda e: e.memset(ssq[:, col:col + 1], 0.0), writes=[("ssq", col)])
            S.op("act", lambda e: e.activation(out=junk[:], in_=xres[:, t, :], func=AF.Square, accum_out=ssq[:, col:col + 1]),
                 reads=[("xres", t), ("ssq", col)], writes=["junk", ("ssq", col)])
            S.op("act", lambda e: e.activation(out=sdv[:, col:col + 1], in_=ssq[:, col:col + 1], func=AF.Sqrt, scale=1.0 / D, bias=epsc),
                 reads=[("ssq", col), "epsc"], writes=[("sdv", col)])
            S.op("dve", lambda e: e.reciprocal(out=rstd[:, col:col + 1], in_=sdv[:, col:col + 1]), reads=[("sdv", col)], writes=[("rstd", col)])

        S.fence()
        h2f = tmpC[:, :].rearrange("p (c t) -> p c t", c=8)
        for t in range(NOWN):
            norm_stats(t, t)
            xs2 = xin[t % 2]
            S.op("act", lambda e, t=t, xs2=xs2: e.activation(out=xs2[:], in_=xres[:, t, :], func=AF.Copy, scale=rstd[:, t:t + 1]),
                 reads=[("xres", t), ("rstd", t)], writes=[("xin", t % 2)])

            def tr(e, xs2=xs2):
                ins = None
                for c in range(8):
                    ins = e.transpose(out=ps[2 + c // 4][:, (c % 4) * 128:(c % 4 + 1) * 128], in_=xs2[:, c * 128:(c + 1) * 128], identity=identf[:])
                return ins
            S.op("pe", tr, reads=[("xin", t % 2), "identf"], writes=[PK(2), PK(3)])
            for hh in range(2):
                S.op("dve", lambda e, hh=hh: e.tensor_tensor(out=tmpB[:, hh * 512:(hh + 1) * 512].rearrange("p (c t) -> p c t", c=4),
                                                            in0=ps[2 + hh][:, :].rearrange("p (c t) -> p c t", c=4),
                                                            in1=bc(A2T[:, hh * 4:(hh + 1) * 4], 4), op=ALU.mult),
                     reads=[PK(2 + hh), "A2T"], writes=["tmpB"])
                S.op("dve", lambda e, hh=hh: e.tensor_tensor(out=h2f[:, hh * 4:(hh + 1) * 4, :],
                                                            in0=tmpB[:, hh * 512:(hh + 1) * 512].rearrange("p (c t) -> p c t", c=4),
                                                            in1=bc(shift2T[:, hh * 4:(hh + 1) * 4], 4), op=ALU.add),
                     reads=["tmpB", "modT"], writes=["tmpC"])
            S.op("act", lambda e, t=t: e.copy(out=h2T[:, :, t * 128:(t + 1) * 128], in_=h2f), reads=["tmpC"], writes=[("h2T", t)])

            def mmr(e):
                ins = None
                for kc in range(8):
                    ins = e.matmul(ps[4][:, 0:NE], lhsT=h2f[:, kc, :], rhs=wrT[:, kc, :], start=(kc == 0), stop=(kc == 7))
                return ins
            S.op("pe", mmr, reads=["tmpC", "wrT"], writes=[PK(4)])
            S.op("dve", lambda e: e.tensor_tensor(out=logit, in0=ps[4][:, 0:NE], in1=brB, op=ALU.add),
                 reads=[PK(4), "brB"], writes=["logit"])
            S.op("dve", lambda e: e.max(out=top8, in_=logit), reads=["logit"], writes=["top8"])
            S.op("dve", lambda e: e.tensor_scalar(out=est[:, 0:1], in0=top8[:, 0:1], scalar1=-1.0, scalar2=None, op0=ALU.mult),
                 reads=["top8"], writes=[("est", 0)])
            S.op("act", lambda e: e.activation(out=rtmp[:, 0, :], in_=logit, func=AF.Exp, bias=est[:, 0:1]),
                 reads=["logit", ("est", 0)], writes=[("rtmp", 0)])
            S.op("dve", lambda e: e.tensor_scalar(out=rtmp[:, 1, :], in0=logit, scalar1=top8[:, 3:4], scalar2=None, op0=ALU.is_ge),
                 reads=["logit", "top8"], writes=[("rtmp", 1)])
            S.op("dve", lambda e: e.tensor_tensor(out=rtmp[:, 2, :], in0=rtmp[:, 0, :], in1=rtmp[:, 1, :], op=ALU.mult),
                 reads=[("rtmp", 0), ("rtmp", 1)], writes=[("rtmp", 2)])
            S.op("dve", lambda e: e.tensor_reduce(out=est[:, 1:2], in_=rtmp[:, 2, :], axis=AX.X, op=ALU.add),
                 reads=[("rtmp", 2)], writes=[("est", 0)])
            S.op("dve", lambda e: e.reciprocal(out=est[:, 2:3], in_=est[:, 1:2]), reads=[("est", 0)], writes=[("est", 0)])
            S.op("dve", lambda e, t=t: e.tensor_scalar(out=gates[:, t, :], in0=rtmp[:, 2, :], scalar1=est[:, 2:3], scalar2=None, op0=ALU.mult),
                 reads=[("rtmp", 2), ("est", 0)], writes=[("gates", t)])
            S.op("pe", lambda e, t=t: e.transpose(out=ps[5][0:NE, 0:128], in_=gates[:, t, :], identity=identf[:]),
                 reads=[("gates", t), "identf"], writes=[PK(5)])
            S.op("act", lambda e: e.copy(out=gT, in_=ps[5][0:NE, 0:128]), reads=[PK(5)], writes=["gT"])
            for nh in range(2):
                S.op("pe", lambda e, nh=nh: e.matmul(ps[nh][:, :], lhsT=gT, rhs=b2s[:, nh * 512:(nh + 1) * 512], start=True, stop=True),
                     reads=["gT", "b2s"], writes=[PK(nh)])
                S.op("dve", lambda e, nh=nh: e.tensor_tensor(out=tmpB[:, nh * 512:(nh + 1) * 512], in0=ps[nh][:, :],
                                                            in1=gate12B[:, 1, nh * 512:(nh + 1) * 512], op=ALU.mult),
                     reads=[PK(nh), "gate12B"], writes=["tmpB"])
                S.op("dve", lambda e, nh=nh, t=t: e.tensor_tensor(out=xres[:, t, nh * 512:(nh + 1) * 512], in0=xres[:, t, nh * 512:(nh + 1) * 512],
                                                                 in1=tmpB[:, nh * 512:(nh + 1) * 512], op=ALU.add),
                     reads=[("xres", t), "tmpB"], writes=[("xres", t)])

        if debug:
            d = dbg_out("gates", [128, NOWN * NE])
            dma("sp", "dbg", d[:, :], gates.rearrange("p t e -> p (t e)"), reads=[("gates", t) for t in range(NOWN)], writes=["dbgout"])

        if stop == 'p3':
            S.fence(); S.emit(); return nc, dbg_outs
        S.fence()
        S.op("dve", lambda e: e.tensor_scalar(out=b1l7s, in0=b1T[:, :, :, 1], scalar1=7.0, scalar2=1.0 / ALPHA, op0=ALU.add, op1=ALU.mult),
             reads=["b1T"], writes=["b1l7s"])
        gsb = [tmpA[:, 0:512], tmpA[:, 512:1024]]
        rlb = [tmpB[:, 0:512], tmpB[:, 512:1024]]
        tbb = [tmpC[:, 0:512], tmpC[:, 512:1024]]
        ucnt = 0
        wcnt = 0
        chunks = [(ex_, fc_) for ex_ in range(n_experts) for fc_ in range(8)]
        issued = [0]

        def issue_upto(n):
            while issued[0] < min(n, len(chunks)):
                ci_ = issued[0]
                ex_, fc_ = chunks[ci_]
                dma("pool", f"w1c{ci_ % 3}", w1c[ci_ % 3][:, :, :], w1[ex_, :, fc_ * 256:(fc_ + 1) * 256].rearrange("(c p) n -> p c n", p=128),
                    writes=[("w1c", ci_ % 3)])
                issued[0] += 1

        for ex in range(n_experts):
            for fc in range(8):
                issue_upto(wcnt + 3)
                if fc == 0:
                    for hh in range(2):
                        dma("pool", "w2b", w2b[:, hh * 4:(hh + 1) * 4, :], w2[ex, hh * 512:(hh + 1) * 512, :].rearrange("(c p) n -> p c n", p=128),
                            writes=["w2b"])
                wb = w1c[wcnt % 3]
                wk = ("w1c", wcnt % 3)
                wcnt += 1
                for tg in range(4):
                    u = ucnt % 2
                    ucnt += 1
                    for gl in range(2):
                        bank = ps[2 * u + gl]

                        def mm(e, bank=bank, wb=wb, gl=gl, tg=tg):
                            ins = None
                            for kc in range(8):
                                ins = e.matmul(bank[:, :], lhsT=wb[:, kc, gl::2], rhs=h2T[:, kc, tg * 512:(tg + 1) * 512],
                                               start=(kc == 0), stop=(kc == 7))
                            return ins
                        S.op("pe", mm, reads=[wk] + [("h2T", t) for t in range(tg * 4, tg * 4 + 4)], writes=[PK(2 * u + gl)])
                    gk, rk = ("gsb", u), ("rlb", u)
                    S.op("dve", lambda e, u=u, ex=ex, fc=fc: e.tensor_scalar(out=gsb[u], in0=ps[2 * u][:, :], scalar1=b1T[:, ex, fc, 0:1], scalar2=7.0,
                                                                             op0=ALU.add, op1=ALU.min),
                         reads=[PK(2 * u), "b1T"], writes=[gk])
                    S.op("act", lambda e, u=u: e.activation(out=gsb[u], in_=gsb[u], func=AF.Silu, scale=ALPHA),
                         reads=[gk], writes=[gk])
                    S.op("act", lambda e, u=u, ex=ex, fc=fc: e.activation(out=rlb[u], in_=ps[2 * u + 1][:, :], func=AF.Relu, scale=1.0 / ALPHA,
                                                                          bias=b1l7s[:, ex, fc:fc + 1]),
                         reads=[PK(2 * u + 1), "b1l7s"], writes=[rk])
                    S.op("dve", lambda e, u=u: e.tensor_scalar(out=rlb[u], in0=rlb[u], scalar1=14.0 / ALPHA, scalar2=-6.0 / ALPHA,
                                                               op0=ALU.min, op1=ALU.add),
                         reads=[rk], writes=[rk])
                    S.op("dve", lambda e, u=u, fc=fc, tg=tg: e.tensor_tensor(out=actT[:, fc, tg * 512:(tg + 1) * 512], in0=gsb[u], in1=rlb[u], op=ALU.mult),
                         reads=[gk, rk], writes=[("yT", fc, t) for t in range(tg * 4, tg * 4 + 4)])
            for t in range(NOWN):
                for nh in range(2):
                    bi = 4 + (2 * t + nh) % 4
                    bank = ps[bi]

                    def mm(e, bank=bank, t=t, nh=nh):
                        ins = None
                        for fc in range(8):
                            ins = e.matmul(bank[:, :], lhsT=actT[:, fc, t * 128:(t + 1) * 128], rhs=w2b[:, fc, nh * 512:(nh + 1) * 512],
                                           start=(fc == 0), stop=(fc == 7))
                        return ins
                    S.op("pe", mm, reads=[("yT", fc, t) for fc in range(8)] + ["w2b"], writes=[PK(bi)])
                    tb = tbb[(2 * t + nh) % 2]
                    tk = ("tbb", (2 * t + nh) % 2)
                    S.op("dve", lambda e, bank=bank, t=t, nh=nh, ex=ex, tb=tb: e.scalar_tensor_tensor(out=tb, in0=bank[:, :], scalar=gates[:, t, ex:ex + 1],
                                                                                                   in1=gate12B[:, 1, nh * 512:(nh + 1) * 512],
                                                                                                   op0=ALU.mult, op1=ALU.mult),
                         reads=[PK(bi), ("gates", t), "gate12B"], writes=[tk])
                    S.op("dve", lambda e, t=t, nh=nh, tb=tb: e.tensor_tensor(out=xres[:, t, nh * 512:(nh + 1) * 512], in0=xres[:, t, nh * 512:(nh + 1) * 512],
                                                                            in1=tb, op=ALU.add),
                         reads=[("xres", t), tk], writes=[("xres", t)])

        if stop == 'moe':
            S.fence(); S.emit(); return nc, dbg_outs
        S.fence()
        nfB = tmpA
        ld("nfB", nfB[:], normfB_d[:, :])
        for t in range(NOWN):
            norm_stats(t, 16 + t)
            ob = xin[t % 2]
            S.op("dve", lambda e, t=t, ob=ob: e.scalar_tensor_tensor(out=ob[:], in0=xres[:, t, :], scalar=rstd[:, 16 + t:17 + t], in1=nfB[:],
                                                                    op0=ALU.mult, op1=ALU.mult),
                 reads=[("xres", t), ("rstd", 16 + t), "nfB"], writes=[("xin", t % 2)])
            dma("sp", f"out{t % 2}", out_d[t * 128:(t + 1) * 128, :], ob[:], reads=[("xin", t % 2)], writes=[("out", t)])
        S.fence()
        S.emit()
    return nc, dbg_outs


_CACHE = {}


def kernel(**inputs):
    inp = {k: np.asarray(v) for k, v in inputs.items()}
    tabs = [const_tables(0), const_tables(1)]
    in_maps = [prep_core(c, inp, tabs) for c in range(8)]
    if "nc" not in _CACHE:
        _CACHE["nc"] = build_program()[0]
    nc = _CACHE["nc"]
    res = run_bass_kernel_spmd(nc, in_maps, core_ids=list(range(8)))
    out = np.zeros((4, NBLK, 128, D), np.float32)
    for c in range(8):
        b, half = c // 2, c % 2
        own = tabs[half][1]
        out[b, own] = np.asarray(res.results[c]["out"]).reshape(NOWN, 128, D)
    return out.reshape(4, SEQ, D)
```
